# Optimizing a Trainium2 kernel written in Bass

```python
import math
import jax
import jax.numpy as jnp
from jax import lax
import numpy as np

D_MODEL = 2048
BATCH = 4
SEQ = 2048
DEPTH = 4

CTX_LEN = 256
GRID_W = 64
NORM_EPS = 1e-6

GM_CHUNK = 128
GM_GROUPS = 4
GM_GROUP_CH = 128
GM_WIDTH = GM_GROUPS * GM_GROUP_CH
GDN_HEADS = 4
GDN_HEAD_DIM = 128
GDN_CONV = 5
GDN_CHUNK = 64
RET_HEADS = 4
RET_KEY_DIM = 128
RET_VAL_DIM = 256
RET_CHUNK = 64
ROPE_BASE = 10000.0
MOE_GROUPS = 4
MOE_EXPERTS_PER_GROUP = 8
MOE_EXPERTS = MOE_GROUPS * MOE_EXPERTS_PER_GROUP
MOE_TOP_K = 2
MOE_HIDDEN = 512
MOE_BLOCK = 128

GM_UV_COLS = 2 * GM_WIDTH
GDN_QKV_COLS = 3 * GDN_HEADS * GDN_HEAD_DIM
GDN_AB_COLS = 2 * GDN_HEADS
GDN_GATE_COLS = GDN_HEADS * GDN_HEAD_DIM
RET_QK_COLS = RET_HEADS * RET_KEY_DIM
RET_V_COLS = RET_HEADS * RET_VAL_DIM
MERGE_COLS = 3 * D_MODEL
REC_START = GM_UV_COLS
REC_SIZES = (GDN_QKV_COLS, GDN_AB_COLS, GDN_AB_COLS, RET_QK_COLS, RET_QK_COLS, RET_V_COLS)
REC_END = REC_START + sum(REC_SIZES)
POST_SIZES = (GDN_GATE_COLS, RET_V_COLS, MERGE_COLS)
IN_COLS = REC_END + sum(POST_SIZES)

kernel_name = 'hybrid_gated_branch_dit_block'


def split_cols(t, sizes):
    idx, acc = [], 0
    for s in sizes[:-1]:
        acc += s
        idx.append(acc)
    return jnp.split(t, idx, axis=-1)


def rms_norm(t, w):
    tf = t.astype(jnp.float32)
    y = tf * lax.rsqrt(jnp.mean(tf * tf, axis=-1, keepdims=True) + NORM_EPS)
    return (y * w).astype(t.dtype)


def standardize(t):
    tf = t.astype(jnp.float32)
    mu = jnp.mean(tf, axis=-1, keepdims=True)
    d = tf - mu
    return d * lax.rsqrt(jnp.mean(d * d, axis=-1, keepdims=True) + NORM_EPS)


def l2_normalize(t):
    return t * lax.rsqrt(jnp.sum(t * t, axis=-1, keepdims=True) + NORM_EPS)


def modulate(h, shift, scale):
    return h * (1.0 + scale) + shift


def flip(t):
    return jnp.flip(t, axis=1)


def centred_dwconv(t, w):
    K = w.shape[0]
    r = K // 2
    L = t.shape[1]
    tp = jnp.pad(t, ((0, 0), (r, r), (0, 0)))
    out = tp[:, 0:L] * w[0]
    for j in range(1, K):
        out = out + tp[:, j:j + L] * w[j]
    return out


def axial_rope(rows, dim):
    n = dim // 4
    freq = ROPE_BASE ** (-jnp.arange(n, dtype=jnp.float32) / n)
    row = jnp.repeat(jnp.arange(rows, dtype=jnp.float32), GRID_W)
    col = (jnp.arange(rows * GRID_W) % GRID_W).astype(jnp.float32)
    ang = jnp.concatenate([row[:, None] * freq, col[:, None] * freq], axis=-1)
    ang = jnp.concatenate([ang, ang], axis=-1)[:, None, :]
    return jnp.cos(ang), jnp.sin(ang)


def apply_rope(t, cos, sin):
    t1, t2 = jnp.split(t, 2, axis=-1)
    return t * cos + jnp.concatenate([-t2, t1], axis=-1) * sin


def to_chunks(t, size):
    B, L, H = t.shape[:3]
    return jnp.moveaxis(t.reshape(B, L // size, size, H, -1), 3, 2)


def from_chunks(o):
    N, B, H, C, d = o.shape
    return jnp.moveaxis(jnp.moveaxis(o, 0, 1), 2, 3).reshape(B, N * C, H, d)


def gated_delta_chunked(q, k, v, g, beta, state0):
    Dk = q.shape[-1]
    Dv = v.shape[-1]
    C = GDN_CHUNK
    qc = to_chunks(q * Dk ** -0.5, C)
    kc = to_chunks(k, C)
    vc = to_chunks(v, C)
    bc = to_chunks(beta[..., None], C)
    gc = jnp.cumsum(to_chunks(g[..., None], C)[..., 0], axis=-1)
    causal = jnp.tril(jnp.ones((C, C), bool))
    strict = jnp.tril(jnp.ones((C, C), bool), -1)
    decay = jnp.exp(jnp.where(causal, gc[..., :, None] - gc[..., None, :], -jnp.inf))
    kb = kc * bc
    a_mat = jnp.where(strict, jnp.einsum('bnhik,bnhjk->bnhij', kb, kc) * decay, 0.0) + jnp.eye(C, dtype=kc.dtype)
    rhs = jnp.concatenate([vc * bc, kb * jnp.exp(gc)[..., None]], axis=-1)
    sol = lax.linalg.triangular_solve(a_mat, rhs, left_side=True, lower=True, unit_diagonal=True)
    u, w = sol[..., :Dv], sol[..., Dv:]
    qk = jnp.einsum('bnhik,bnhjk->bnhij', qc, kc) * decay

    def step(S, xs):
        q_i, k_i, u_i, w_i, g_i, qk_i = xs
        v_new = u_i - jnp.einsum('bhck,bhkv->bhcv', w_i, S)
        o_i = (jnp.einsum('bhck,bhkv->bhcv', q_i * jnp.exp(g_i)[..., None], S)
               + jnp.einsum('bhij,bhjv->bhiv', qk_i, v_new))
        g_last = g_i[..., -1:]
        S = (S * jnp.exp(g_last)[..., None]
             + jnp.einsum('bhck,bhcv->bhkv', k_i * jnp.exp(g_last - g_i)[..., None], v_new))
        return S, o_i

    xs = tuple(jnp.moveaxis(t, 1, 0) for t in (qc, kc, u, w, gc, qk))
    S, o = lax.scan(step, state0, xs)
    return from_chunks(o), S


def retention_chunked(q, k, v, log_gamma, state0):
    C = RET_CHUNK
    qc, kc, vc = to_chunks(q, C), to_chunks(k, C), to_chunks(v, C)
    pos = jnp.arange(C, dtype=jnp.float32)
    diff = pos[:, None] - pos[None, :]
    lg = log_gamma[:, None, None]
    decay = jnp.exp(jnp.where(diff >= 0, lg * diff, -jnp.inf))
    cross_decay = jnp.exp(log_gamma[:, None] * (pos + 1.0))[..., None]
    in_decay = jnp.exp(log_gamma[:, None] * (C - 1.0 - pos))[..., None]
    chunk_decay = jnp.exp(log_gamma * C)[:, None, None]
    inner = jnp.einsum('bnhij,bnhjv->bnhiv', jnp.einsum('bnhik,bnhjk->bnhij', qc, kc) * decay, vc)

    def step(S, xs):
        q_i, k_i, v_i = xs
        cross = jnp.einsum('bhck,bhkv->bhcv', q_i, S) * cross_decay
        S = S * chunk_decay + jnp.einsum('bhck,bhcv->bhkv', k_i * in_decay, v_i)
        return S, cross

    S, cross = lax.scan(step, state0, tuple(jnp.moveaxis(t, 1, 0) for t in (qc, kc, vc)))
    return from_chunks(jnp.moveaxis(inner, 1, 0) + cross), S


def recurrent_passes(rec, conv_w, a_log, dt_bias, log_gamma, rope, init):
    gdn_qkv, gdn_a, gdn_b, ret_q, ret_k, ret_v = rec
    B, L, _ = gdn_qkv.shape
    f32 = jnp.float32
    qkv = jax.nn.silu(centred_dwconv(gdn_qkv, conv_w)).astype(f32)
    gq, gk, gv = [t.reshape(B, L, GDN_HEADS, GDN_HEAD_DIM) for t in jnp.split(qkv, 3, axis=-1)]
    gq, gk = l2_normalize(gq), l2_normalize(gk)
    a = gdn_a.astype(f32).reshape(B, L, 2, GDN_HEADS)
    log_decay = -jnp.exp(a_log.astype(f32)) * jax.nn.softplus(a + dt_bias.astype(f32))
    beta = jax.nn.sigmoid(gdn_b.astype(f32).reshape(B, L, 2, GDN_HEADS))
    o_gf, s_gf = gated_delta_chunked(gq, gk, gv, log_decay[:, :, 0], beta[:, :, 0], init[0])
    o_gb, s_gb = gated_delta_chunked(flip(gq), flip(gk), flip(gv), flip(log_decay[:, :, 1]),
                                     flip(beta[:, :, 1]), init[1])
    o_gdn = o_gf + flip(o_gb)

    rq = ret_q.astype(f32).reshape(B, L, RET_HEADS, RET_KEY_DIM)
    rk = ret_k.astype(f32).reshape(B, L, RET_HEADS, RET_KEY_DIM)
    rv = ret_v.astype(f32).reshape(B, L, RET_HEADS, RET_VAL_DIM)
    if rope is not None:
        rq, rk = apply_rope(rq, *rope), apply_rope(rk, *rope)
    rq = rq * RET_KEY_DIM ** -0.5
    o_rf, s_rf = retention_chunked(rq, rk, rv, log_gamma[0], init[2])
    o_rb, s_rb = retention_chunked(flip(rq), flip(rk), flip(rv), log_gamma[1], init[3])
    return o_gdn, o_rf + flip(o_rb), (s_gf, s_gb, s_rf, s_rb)


def chunk_spatial_gating(uv, norm_w, w_s, b_s):
    B, L, _ = uv.shape
    u, v = jnp.split(jax.nn.gelu(uv, approximate=False), 2, axis=-1)
    v = (standardize(v) * norm_w).astype(uv.dtype)
    v = v.reshape(B, L // GM_CHUNK, GM_CHUNK, GM_GROUPS, GM_GROUP_CH)
    s = jnp.einsum('gpq,bnqgc->bnpgc', w_s, v) + b_s.T[:, :, None]
    return u * s.reshape(B, L, GM_WIDTH)


def merge_branches(p, o_gdn, o_ret, gm_norm_w, gm_w, gm_b, gdn_norm_w, ret_norm_w,
                   w_br_gm, w_br_gdn, w_br_ret, w_out):
    B, L, _ = p.shape
    dt = p.dtype
    uv = p[..., :REC_START]
    gdn_gate, ret_gate, merge_logits = split_cols(p[..., REC_END:], POST_SIZES)
    y_gm = chunk_spatial_gating(uv, gm_norm_w, gm_w, gm_b) @ w_br_gm
    og = o_gdn * lax.rsqrt(jnp.mean(o_gdn * o_gdn, axis=-1, keepdims=True) + NORM_EPS) * gdn_norm_w
    y_gdn = (og.reshape(B, L, -1).astype(dt) * jax.nn.silu(gdn_gate)) @ w_br_gdn
    orr = standardize(o_ret).reshape(B, L, -1) * ret_norm_w
    y_ret = (orr.astype(dt) * jax.nn.silu(ret_gate)) @ w_br_ret
    s_gm, s_gdn, s_ret = jnp.split(jax.nn.sigmoid(merge_logits), 3, axis=-1)
    return (s_gm * y_gm + s_gdn * y_gdn + s_ret * y_ret) @ w_out


def routed_experts(h, expert_idx, weights, w_gate, w_up, w_down):
    T, D = h.shape
    K = expert_idx.shape[1]
    E = w_gate.shape[0]
    A = T * K
    n_blocks = -(-A // MOE_BLOCK) + E
    n_slots = n_blocks * MOE_BLOCK
    flat_e = expert_idx.reshape(A)
    order = jnp.argsort(flat_e)
    e_sorted = flat_e[order]
    tok_sorted = (order // K).astype(jnp.int32)
    w_sorted = weights.reshape(A)[order]
    counts = jnp.bincount(flat_e, length=E)
    padded = (counts + MOE_BLOCK - 1) // MOE_BLOCK * MOE_BLOCK
    pad_end = jnp.cumsum(padded)
    dest = (pad_end - padded)[e_sorted] + jnp.arange(A) - (jnp.cumsum(counts) - counts)[e_sorted]
    slot_tok = jnp.full((n_slots,), T, jnp.int32).at[dest].set(tok_sorted)
    slot_w = jnp.zeros((n_slots,), weights.dtype).at[dest].set(w_sorted)
    block_expert = jnp.minimum(jnp.searchsorted(pad_end, jnp.arange(n_blocks) * MOE_BLOCK, side='right'), E - 1)
    h_pad = jnp.concatenate([h, jnp.zeros((1, D), h.dtype)], axis=0)

    def expert_block(args):
        tok, e = args
        xb = h_pad[tok]
        act = jax.nn.silu(xb @ w_gate[e]) * (xb @ w_up[e])
        return act @ w_down[e]

    y = lax.map(expert_block, (slot_tok.reshape(n_blocks, MOE_BLOCK), block_expert))
    y = y.reshape(n_slots, D) * slot_w[:, None].astype(y.dtype)
    return jnp.zeros((T + 1, D), y.dtype).at[slot_tok].add(y)[:T]


def hier_moe(h, wg, bg, we, be, w_gate, w_up, w_down):
    T, D = h.shape
    hf = h.astype(jnp.float32)
    p_group = jax.nn.softmax(hf @ wg.astype(jnp.float32) + bg.astype(jnp.float32), axis=-1)
    pg_top, grp = lax.top_k(p_group, 1)
    logits_e = (hf @ we.astype(jnp.float32) + be.astype(jnp.float32)).reshape(T, MOE_GROUPS, MOE_EXPERTS_PER_GROUP)
    sel = jnp.broadcast_to(grp[:, :, None], (T, 1, MOE_EXPERTS_PER_GROUP))
    p_e = jax.nn.softmax(jnp.take_along_axis(logits_e, sel, axis=1)[:, 0], axis=-1)
    pe_top, e_top = lax.top_k(p_e, MOE_TOP_K)
    weights = pg_top * pe_top / jnp.sum(pe_top, axis=-1, keepdims=True)
    expert_idx = grp * MOE_EXPERTS_PER_GROUP + e_top
    return routed_experts(h, expert_idx, weights, w_gate, w_up, w_down)


def setup_inputs(seed: int = 0) -> dict:
    key = jax.random.key(seed)
    ks = iter(jax.random.split(key, 40))
    f32 = jnp.float32

    def nrm(shape, scale):
        return jax.random.normal(next(ks), shape, f32) * scale

    x = nrm((BATCH, SEQ, D_MODEL), 1.0)
    c = nrm((BATCH, D_MODEL), 1.0)
    ctx = nrm((BATCH, CTX_LEN, D_MODEL), 1.0)
    c_ctx = nrm((D_MODEL,), 1.0)
    mod_w = nrm((DEPTH, D_MODEL, 6 * D_MODEL), 0.5 * D_MODEL ** -0.5)
    mod_b = nrm((DEPTH, 6 * D_MODEL), 0.02)
    norm1_w = 1.0 + nrm((DEPTH, D_MODEL), 0.02)
    w_in = nrm((DEPTH, D_MODEL, IN_COLS), D_MODEL ** -0.5)
    gm_norm_w = 1.0 + nrm((DEPTH, GM_WIDTH), 0.02)
    gm_spatial_w = nrm((DEPTH, GM_GROUPS, GM_CHUNK, GM_CHUNK), GM_CHUNK ** -0.5)
    gm_spatial_b = 1.0 + nrm((DEPTH, GM_GROUPS, GM_CHUNK), 0.02)
    gdn_conv_w = nrm((DEPTH, GDN_CONV, GDN_QKV_COLS), GDN_CONV ** -0.5)
    gdn_a_log = jnp.log(jax.random.uniform(next(ks), (DEPTH, 2, GDN_HEADS), f32, 1.0, 16.0))
    dt = jnp.exp(jax.random.uniform(next(ks), (DEPTH, 2, GDN_HEADS), f32, math.log(1e-3), math.log(1e-1)))
    gdn_dt_bias = dt + jnp.log(-jnp.expm1(-dt))
    gdn_norm_w = 1.0 + nrm((DEPTH, GDN_HEAD_DIM), 0.02)
    gamma0 = 1.0 - 2.0 ** (-5.0 - jnp.arange(RET_HEADS, dtype=f32))
    ret_decay_logit = jnp.log(gamma0 / (1.0 - gamma0)) + nrm((DEPTH, 2, RET_HEADS), 0.1)
    ret_norm_w = 1.0 + nrm((DEPTH, RET_V_COLS), 0.02)
    w_br_gm = nrm((DEPTH, GM_WIDTH, D_MODEL), GM_WIDTH ** -0.5)
    w_br_gdn = nrm((DEPTH, GDN_GATE_COLS, D_MODEL), GDN_GATE_COLS ** -0.5)
    w_br_ret = nrm((DEPTH, RET_V_COLS, D_MODEL), RET_V_COLS ** -0.5)
    w_out = nrm((DEPTH, D_MODEL, D_MODEL), D_MODEL ** -0.5)
    norm2_w = 1.0 + nrm((DEPTH, D_MODEL), 0.02)
    router_group_w = nrm((DEPTH, D_MODEL, MOE_GROUPS), D_MODEL ** -0.5)
    router_group_b = nrm((DEPTH, MOE_GROUPS), 0.01)
    router_expert_w = nrm((DEPTH, D_MODEL, MOE_EXPERTS), D_MODEL ** -0.5)
    router_expert_b = nrm((DEPTH, MOE_EXPERTS), 0.01)
    moe_w_gate = nrm((DEPTH, MOE_EXPERTS, D_MODEL, MOE_HIDDEN), D_MODEL ** -0.5)
    moe_w_up = nrm((DEPTH, MOE_EXPERTS, D_MODEL, MOE_HIDDEN), D_MODEL ** -0.5)
    moe_w_down = nrm((DEPTH, MOE_EXPERTS, MOE_HIDDEN, D_MODEL), MOE_HIDDEN ** -0.5)
    final_norm_w = 1.0 + nrm((D_MODEL,), 0.02)
    return {'x': x, 'c': c, 'ctx': ctx, 'c_ctx': c_ctx, 'mod_w': mod_w, 'mod_b': mod_b,
            'norm1_w': norm1_w, 'w_in': w_in, 'gm_norm_w': gm_norm_w, 'gm_spatial_w': gm_spatial_w,
            'gm_spatial_b': gm_spatial_b, 'gdn_conv_w': gdn_conv_w, 'gdn_a_log': gdn_a_log,
            'gdn_dt_bias': gdn_dt_bias, 'gdn_norm_w': gdn_norm_w, 'ret_decay_logit': ret_decay_logit,
            'ret_norm_w': ret_norm_w, 'w_br_gm': w_br_gm, 'w_br_gdn': w_br_gdn, 'w_br_ret': w_br_ret,
            'w_out': w_out, 'norm2_w': norm2_w, 'router_group_w': router_group_w,
            'router_group_b': router_group_b, 'router_expert_w': router_expert_w,
            'router_expert_b': router_expert_b, 'moe_w_gate': moe_w_gate, 'moe_w_up': moe_w_up,
            'moe_w_down': moe_w_down, 'final_norm_w': final_norm_w}


def reference(x, c, ctx, c_ctx, mod_w, mod_b, norm1_w, w_in, gm_norm_w, gm_spatial_w, gm_spatial_b,
              gdn_conv_w, gdn_a_log, gdn_dt_bias, gdn_norm_w, ret_decay_logit, ret_norm_w,
              w_br_gm, w_br_gdn, w_br_ret, w_out, norm2_w, router_group_w, router_group_b,
              router_expert_w, router_expert_b, moe_w_gate, moe_w_up, moe_w_down, final_norm_w):
    B, L, D = x.shape
    rows = L // GRID_W
    rope = axial_rope(rows, RET_KEY_DIM)
    gdn_zero = jnp.zeros((B, GDN_HEADS, GDN_HEAD_DIM, GDN_HEAD_DIM), jnp.float32)
    ret_zero = jnp.zeros((B, RET_HEADS, RET_KEY_DIM, RET_VAL_DIM), jnp.float32)
    zero_states = (gdn_zero, gdn_zero, ret_zero, ret_zero)
    s = ctx
    for l in range(DEPTH):
        last = l == DEPTH - 1
        mod = jax.nn.silu(c) @ mod_w[l] + mod_b[l]
        mod_c = jax.nn.silu(c_ctx) @ mod_w[l] + mod_b[l]
        sh1, sc1, gt1, sh2, sc2, gt2 = jnp.split(mod[:, None, :], 6, axis=-1)
        csh1, csc1, cgt1, csh2, csc2, cgt2 = jnp.split(mod_c[None, None, :], 6, axis=-1)
        log_gamma = jax.nn.log_sigmoid(ret_decay_logit[l].astype(jnp.float32))
        rec_params = (gdn_conv_w[l], gdn_a_log[l], gdn_dt_bias[l], log_gamma)
        merge_params = (gm_norm_w[l], gm_spatial_w[l], gm_spatial_b[l], gdn_norm_w[l], ret_norm_w[l],
                        w_br_gm[l], w_br_gdn[l], w_br_ret[l], w_out[l])

        hc = modulate(rms_norm(s, norm1_w[l]), csh1, csc1)
        if last:
            rec_c = split_cols(hc @ w_in[l, :, REC_START:REC_END], REC_SIZES)
        else:
            pc = hc @ w_in[l]
            rec_c = split_cols(pc[..., REC_START:REC_END], REC_SIZES)
        o_gdn_c, o_ret_c, ctx_states = recurrent_passes(rec_c, *rec_params, None, zero_states)

        h = modulate(rms_norm(x, norm1_w[l]), sh1, sc1)
        p = h @ w_in[l]
        o_gdn, o_ret, _ = recurrent_passes(split_cols(p[..., REC_START:REC_END], REC_SIZES),
                                           *rec_params, rope, ctx_states)
        x = x + gt1 * merge_branches(p, o_gdn, o_ret, *merge_params)
        if not last:
            s = s + cgt1 * merge_branches(pc, o_gdn_c, o_ret_c, *merge_params)

        moe_params = (router_group_w[l], router_group_b[l], router_expert_w[l], router_expert_b[l],
                      moe_w_gate[l], moe_w_up[l], moe_w_down[l])
        h2 = modulate(rms_norm(x, norm2_w[l]), sh2, sc2).reshape(B * L, D)
        if last:
            x = x + gt2 * hier_moe(h2, *moe_params).reshape(B, L, D)
        else:
            h2c = modulate(rms_norm(s, norm2_w[l]), csh2, csc2).reshape(-1, D)
            y2 = hier_moe(jnp.concatenate([h2, h2c], axis=0), *moe_params)
            x = x + gt2 * y2[:B * L].reshape(B, L, D)
            s = s + cgt2 * y2[B * L:].reshape(B, -1, D)
    return rms_norm(x, final_norm_w)
```

```python
import contextlib
import numpy as np
import concourse.bass as bass
import concourse.mybir as mybir
from concourse.bass_utils import run_bass_kernel_spmd

F32 = mybir.dt.float32
BF16 = mybir.dt.bfloat16
I32 = mybir.dt.int32
AF = mybir.ActivationFunctionType
ALU = mybir.AluOpType
AX = mybir.AxisListType

ENGS = ("pe", "act", "dve", "pool", "sp")
SEM_MAX = 30000
NDMA = 6


def _key(h):
    if isinstance(h, (str, int)):
        return h
    if isinstance(h, tuple):
        return tuple(_key(x) for x in h)
    return h.name


class Dep:
    __slots__ = ("w", "r")

    def __init__(self):
        self.w = None
        self.r = []


class Prog:
    def __init__(self, same_engine_sync=True, num_devices=None):
        if num_devices is None:
            self.nc = bass.Bass("TRN2", target_bir_lowering=False)
        else:
            self.nc = bass.Bass("TRN2", target_bir_lowering=False, num_devices=num_devices)
        self.es = contextlib.ExitStack()
        self.streams = {e: [] for e in ENGS}
        self.cnt = {e: 0 for e in ENGS}
        self.semi = {e: 0 for e in ENGS}
        self.sem = {}
        self.waited = {e: {} for e in ENGS}
        self.deps = {}
        self.dma_sems = {}
        self.dma_cnt = {}
        self.dma_rr = {e: 0 for e in ENGS}
        self.same_engine_sync = same_engine_sync
        self.nsem = 0
        self.ntile = 0
        self.all_tokens = {}
        for e in ENGS:
            self._new_eng_sem(e)
        for e in ("sp", "pool", "act"):
            self.dma_sems[e] = []
            for i in range(NDMA):
                s = self._sem(f"d_{e}_{i}")
                self.dma_sems[e].append(s)
                self.dma_cnt[s] = 0

    def _sem(self, name):
        self.nsem += 1
        return self.es.enter_context(self.nc.semaphore(name))

    def _new_eng_sem(self, e):
        self.sem[e] = self._sem(f"s_{e}_{self.semi[e]}")
        self.semi[e] += 1
        self.cnt[e] = 0

    def sb(self, shape, dt=F32, name=None):
        self.ntile += 1
        name = name or f"t{self.ntile}"
        return self.es.enter_context(self.nc.sbuf_tensor(name, list(shape), dt))

    def ps(self, shape, dt=F32, name=None):
        self.ntile += 1
        name = name or f"p{self.ntile}"
        return self.es.enter_context(self.nc.psum_tensor(name, list(shape), dt))

    def dram(self, name, shape, dt=F32, kind="Internal"):
        return self.nc.dram_tensor(name, list(shape), dt, kind=kind)

    def _collect(self, eng, reads, writes):
        waits = {}

        def add(tok):
            if tok is None:
                return
            s, v = tok
            if waits.get(s, 0) < v:
                waits[s] = v

        for h in reads:
            k = _key(h)
            d = self.deps.get(k)
            if d is not None:
                add(d.w)
                if isinstance(k, str) and k.startswith("psum"):
                    for t in d.r:
                        add(t)
        for h in writes:
            d = self.deps.get(_key(h))
            if d is not None:
                add(d.w)
                for t in d.r:
                    add(t)
        out = []
        wd = self.waited[eng]
        for s, v in waits.items():
            if wd.get(s, 0) >= v:
                continue
            if eng == "pe" and s is self.sem["pe"]:
                continue
            if (not self.same_engine_sync) and s is self.sem.get(eng):
                continue
            wd[s] = v
            out.append((s, v))
        return out

    def _record(self, tok, reads, writes):
        for h in reads:
            h = _key(h)
            d = self.deps.get(h)
            if d is None:
                d = self.deps[h] = Dep()
            d.r.append(tok)
            if len(d.r) > 64:
                m = {}
                for s, v in d.r:
                    if m.get(s, 0) < v:
                        m[s] = v
                d.r = list(m.items())
        for h in writes:
            h = _key(h)
            d = self.deps.get(h)
            if d is None:
                d = self.deps[h] = Dep()
            d.w = tok
            d.r = []
        self.all_tokens[tok[0]] = max(self.all_tokens.get(tok[0], 0), tok[1])

    def op(self, eng, fn, reads=(), writes=()):
        ws = self._collect(eng, reads, writes)
        if self.cnt[eng] >= SEM_MAX:
            self._new_eng_sem(eng)
        self.cnt[eng] += 1
        tok = (self.sem[eng], self.cnt[eng])
        self.streams[eng].append((ws, fn, (self.sem[eng], 1)))
        self._record(tok, reads, writes)
        return tok

    def i(self, eng, method, reads, writes, *args, **kw):
        return self.op(eng, lambda e: getattr(e, method)(*args, **kw), reads=reads, writes=writes)

    def mm(self, out, lhsT, rhs, start, stop, reads, writes):
        return self.op("pe", lambda e: e.matmul(out, lhsT, rhs, start=start, stop=stop), reads=reads, writes=writes)

    def dma(self, q, out, in_, reads=(), writes=(), **kw):
        ws = self._collect(q, reads, writes)
        i = self.dma_rr[q] % NDMA
        self.dma_rr[q] += 1
        s = self.dma_sems[q][i]
        prev = self.dma_cnt[s]
        if prev > 0 and self.waited[q].get(s, 0) < prev:
            self.waited[q][s] = prev
            ws.append((s, prev))
        if prev + 16 > SEM_MAX:
            s = self._sem(f"d_{q}_{i}_{self.nsem}")
            self.dma_sems[q][i] = s
            self.dma_cnt[s] = 0
            prev = 0
        self.dma_cnt[s] = prev + 16
        tok = (s, prev + 16)
        self.streams[q].append((ws, lambda e: e.dma_start(out=out, in_=in_, **kw), (s, 16)))
        self._record(tok, reads, writes)
        return tok

    def finish(self):
        nc = self.nc
        final = [(s, v) for s, v in self.all_tokens.items()]
        streams = self.streams

        def replay(name, e):
            for ws, fn, inc in streams[name]:
                for s, v in ws:
                    e.wait_ge(s, v)
                ins = fn(e)
                ins.then_inc(inc[0], inc[1])
            if name == "sp":
                for s, v in final:
                    e.wait_ge(s, v)

        with nc.Block() as block:
            @block.sync
            def _(e):
                replay("sp", e)

            @block.scalar
            def _(e):
                replay("act", e)

            @block.vector
            def _(e):
                replay("dve", e)

            @block.gpsimd
            def _(e):
                replay("pool", e)

            @block.tensor
            def _(e):
                replay("pe", e)
        self.es.close()
        return nc

    def stats(self):
        return {e: len(self.streams[e]) for e in ENGS}


D = 2048
NB = 4
SEQ = 2048
DEPTH = 4
CTX = 256
NT = 1152
KC = 16
EPS = 1e-6
TT3 = [(0, 128), (128, 512), (640, 512)]
IN_COLS = 12304
REC0, REC1 = 1024, 4624
GATE0 = 4624
RGATE0 = 5136
MERGE0 = 6160

_PS = None


class PsumRing:
    def __init__(self, P, n=8):
        self.t = [P.ps([128, 512], F32, name=f"psum{i}") for i in range(n)]
        self.i = 0

    def get(self):
        t = self.t[self.i % len(self.t)]
        self.i += 1
        return t


def colmajor(v):
    v = np.asarray(v, np.float32)
    return np.ascontiguousarray(v.reshape(-1, 128).T)


def emit_norm_mod(P, R, xT, hT, ones, vec, iw, isc_l, ish_l, isc_c, ish_c, scratch, rstd, scl):
    P.op("dve", lambda e: e.scalar_tensor_tensor(out=scl[:, 0:16], in0=vec[:, isc_l * 16:isc_l * 16 + 16], scalar=1.0,
                                                  in1=vec[:, iw * 16:iw * 16 + 16], op0=ALU.add, op1=ALU.mult),
         reads=[vec], writes=[(scl, 0)])
    P.op("dve", lambda e: e.scalar_tensor_tensor(out=scl[:, 16:32], in0=vec[:, isc_c * 16:isc_c * 16 + 16], scalar=1.0,
                                                  in1=vec[:, iw * 16:iw * 16 + 16], op0=ALU.add, op1=ALU.mult),
         reads=[vec], writes=[(scl, 1)])
    pss = [R.get() for _ in range(3)]
    for kc in range(KC):
        sq = scratch[kc % 2]
        P.op("act", lambda e, sq=sq, kc=kc: e.activation(out=sq[:], in_=xT[:, kc, :], func=AF.Square),
             reads=[(xT, kc)], writes=[sq])
        for ti, (o, n) in enumerate(TT3):
            P.op("pe", lambda e, sq=sq, ti=ti, o=o, n=n, kc=kc: e.matmul(pss[ti][:, :n], ones[:], sq[:, o:o + n],
                                                                     start=(kc == 0), stop=(kc == KC - 1)),
                 reads=[sq, ones], writes=[pss[ti]])
    for ti, (o, n) in enumerate(TT3):
        P.op("act", lambda e, ti=ti, o=o, n=n: e.activation(out=rstd[:, o:o + n], in_=pss[ti][:, :n], func=AF.Sqrt,
                                                          bias=EPS, scale=1.0 / D),
             reads=[pss[ti]], writes=[(rstd, ti)])
        P.op("dve", lambda e, o=o, n=n: e.reciprocal(out=rstd[:, o:o + n], in_=rstd[:, o:o + n]),
             reads=[(rstd, ti)], writes=[(rstd, ti)])
    for kc in range(KC):
        tmp = scratch[kc % 2]
        P.op("dve", lambda e, tmp=tmp, kc=kc: e.tensor_tensor(out=tmp[:], in0=xT[:, kc, :], in1=rstd[:], op=ALU.mult),
             reads=[(xT, kc), (rstd, 0), (rstd, 1), (rstd, 2)], writes=[tmp])
        P.op("act", lambda e, tmp=tmp, kc=kc: e.activation(out=hT[:, kc, 0:128], in_=tmp[:, 0:128], func=AF.Identity,
                                                         bias=vec[:, ish_c * 16 + kc:ish_c * 16 + kc + 1],
                                                         scale=scl[:, 16 + kc:17 + kc]),
             reads=[tmp, vec, (scl, 1)], writes=[(hT, kc, 0)])
        P.op("act", lambda e, tmp=tmp, kc=kc: e.activation(out=hT[:, kc, 128:NT], in_=tmp[:, 128:NT], func=AF.Identity,
                                                         bias=vec[:, ish_l * 16 + kc:ish_l * 16 + kc + 1],
                                                         scale=scl[:, kc:kc + 1]),
             reads=[tmp, vec, (scl, 0)], writes=[(hT, kc, 1)])


def hT_reads(hT):
    return [(hT, kc, j) for kc in range(KC) for j in range(2)]


class WStream:
    def __init__(self, P, ncols, nbuf=2, dt=BF16, name="ws"):
        self.P = P
        self.ncols = ncols
        self.bufs = [P.sb([128, KC, ncols], dt, name=f"{name}{i}") for i in range(nbuf)]
        self.i = 0

    def load(self, w_ap, c0, n, kchunks=KC, q="pool"):
        b = self.bufs[self.i % len(self.bufs)]
        self.i += 1
        src = w_ap[:, c0:c0 + n].rearrange("(kc p) n -> p kc n", p=128)
        step = 4
        for k0 in range(0, kchunks, step):
            k1 = min(kchunks, k0 + step)
            self.P.dma(q, b[:, k0:k1, 0:n], src[:, k0:k1, :], writes=[(b, k0)])
        return b

    @staticmethod
    def reads(b, kchunks=KC):
        return [(b, k0) for k0 in range(0, kchunks, 4)]


def build_p1(nv=8):
    P = Prog()
    nc = P.nc
    xT_a = nc.dram_tensor("xT", [nv, D, NT], F32, kind="ExternalInput").ap()
    vec_a = nc.dram_tensor("vec", [nv, 128, 5 * 16], F32, kind="ExternalInput").ap()
    w_d = nc.dram_tensor("w_rec", [D, 3600], F32, kind="ExternalInput").ap()
    cos_a = nc.dram_tensor("cosT", [2, 128, 1024], F32, kind="ExternalInput").ap()
    sin_a = nc.dram_tensor("sinT", [2, 128, 1024], F32, kind="ExternalInput").ap()
    perm_d = nc.dram_tensor("perm", [128, 128], F32, kind="ExternalInput").ap()
    qkv_a = nc.dram_tensor("qkvT", [nv, 1536, NT], F32, kind="ExternalOutput").ap()
    ab_a = nc.dram_tensor("abT", [nv, 16, NT], F32, kind="ExternalOutput").ap()
    rq_a = nc.dram_tensor("rqT", [nv, 512, NT], F32, kind="ExternalOutput").ap()
    rk_a = nc.dram_tensor("rkT", [nv, 512, NT], F32, kind="ExternalOutput").ap()
    rv_a = nc.dram_tensor("rv", [nv, NT, 1024], F32, kind="ExternalOutput").ap()

    R = PsumRing(P)
    xT = P.sb([128, KC, NT], F32, name="xT_sb")
    hT = P.sb([128, KC, NT], BF16, name="hT_sb")
    vec = P.sb([128, 80], F32, name="vec_sb")
    scl = P.sb([128, 32], F32, name="scl")
    ones = P.sb([128, 128], F32, name="ones")
    perm = P.sb([128, 128], F32, name="perm_sb")
    cosT = P.sb([128, 1024], F32, name="cos_sb")
    sinT = P.sb([128, 1024], F32, name="sin_sb")
    scratch = [P.sb([128, NT], F32, name=f"scr{i}") for i in range(2)]
    rstd = P.sb([128, NT], F32, name="rstd")
    P.op("pool", lambda e: e.memset(ones[:], 1.0), writes=[ones])
    P.dma("sp", perm[:], perm_d, writes=[perm])
    ws = WStream(P, 512)
    outb = [P.sb([128, NT], F32, name=f"outb{i}") for i in range(3)]
    rtmp = P.sb([128, NT], F32, name="rtmp")
    tvb = [P.sb([128, 512], F32, name=f"tvb{i}") for i in range(2)]
    for v in range(nv):
        _p1_body(P, R, v, xT_a[v], vec_a[v], w_d, cos_a[v % 2], sin_a[v % 2], qkv_a[v], ab_a[v], rq_a[v], rk_a[v], rv_a[v],
                 xT, hT, vec, scl, ones, perm, cosT, sinT, scratch, rstd, ws, outb, rtmp, tvb)
    return P


def _p1_body(P, R, v, xT_d, vec_d, w_d, cos_d, sin_d, qkv_o, ab_o, rq_o, rk_o, rv_o,
             xT, hT, vec, scl, ones, perm, cosT, sinT, scratch, rstd, ws, outb, rtmp, tvb):
    P.dma("sp", vec[:], vec_d, writes=[vec])
    P.dma("sp", cosT[:], cos_d, writes=[cosT])
    P.dma("sp", sinT[:], sin_d, writes=[sinT])
    xsrc = xT_d.rearrange("(kc p) n -> p kc n", p=128)
    for kc in range(KC):
        P.dma("sp", xT[:, kc, :], xsrc[:, kc, :], writes=[(xT, kc)])
    emit_norm_mod(P, R, xT, hT, ones, vec, 0, 1, 2, 3, 4, scratch, rstd, scl)

    hr = hT_reads(hT)
    oi = [0]

    def fm_chunk(wb, cc, ncols=128):
        ob = outb[oi[0] % 3]
        oi[0] += 1
        for ti, (o, n) in enumerate(TT3):
            ps = R.get()
            for kc in range(KC):
                P.op("pe", lambda e, ps=ps, kc=kc, o=o, n=n: e.matmul(ps[:ncols, :n], wb[:, kc, cc * 128:cc * 128 + ncols],
                                                                    hT[:, kc, o:o + n], start=(kc == 0), stop=(kc == KC - 1)),
                     reads=WStream.reads(wb) + hr, writes=[ps])
            P.op("act", lambda e, ps=ps, o=o, n=n, ob=ob: e.copy(out=ob[:ncols, o:o + n], in_=ps[:ncols, :n]),
                 reads=[ps], writes=[(ob, ti)])
        return ob

    def ob_reads(ob):
        return [(ob, 0), (ob, 1), (ob, 2)]

    for blk in range(3):
        wb = ws.load(w_d, blk * 512, 512)
        for cc in range(4):
            ob = fm_chunk(wb, cc)
            c = blk * 4 + cc
            P.dma("sp", qkv_o[c * 128:(c + 1) * 128, :], ob[:, :], reads=ob_reads(ob))
    wb = ws.load(w_d, 1536, 16)
    ob = fm_chunk(wb, 0, ncols=16)
    P.dma("sp", ab_o[:, :], ob[:16, :], reads=ob_reads(ob))
    for qi, (c0, dst) in enumerate(((1552, rq_o), (2064, rk_o))):
        wb = ws.load(w_d, c0, 512)
        sc = (128.0 ** -0.5) if qi == 0 else 1.0
        for cc in range(4):
            ob = fm_chunk(wb, cc)
            for (o, n) in ((128, 512), (640, 512)):
                ps = R.get()
                P.op("pe", lambda e, ps=ps, o=o, n=n, ob=ob: e.matmul(ps[:, :n], perm[:], ob[:, o:o + n], start=True, stop=True),
                     reads=ob_reads(ob) + [perm], writes=[ps])
                P.op("dve", lambda e, ps=ps, o=o, n=n: e.tensor_tensor(out=rtmp[:, o:o + n], in0=ps[:, :n],
                                                                     in1=sinT[:, o - 128:o - 128 + n], op=ALU.mult),
                     reads=[ps, sinT], writes=[(rtmp, o)])
                P.op("pool", lambda e, o=o, n=n, ob=ob: e.tensor_tensor(out=ob[:, o:o + n], in0=ob[:, o:o + n],
                                                                      in1=cosT[:, o - 128:o - 128 + n], op=ALU.mult),
                     reads=ob_reads(ob) + [cosT], writes=ob_reads(ob))
                P.op("dve", lambda e, o=o, n=n, ob=ob: e.tensor_tensor(out=ob[:, o:o + n], in0=ob[:, o:o + n],
                                                                     in1=rtmp[:, o:o + n], op=ALU.add),
                     reads=ob_reads(ob) + [(rtmp, o)], writes=ob_reads(ob))
            if sc != 1.0:
                P.op("act", lambda e, ob=ob, sc=sc: e.mul(out=ob[:, :], in_=ob[:, :], mul=sc), reads=ob_reads(ob), writes=ob_reads(ob))
            P.dma("sp", dst[cc * 128:(cc + 1) * 128, :], ob[:, :], reads=ob_reads(ob))
    ti_ = 0
    for blk in range(2):
        wb = ws.load(w_d, 2576 + blk * 512, 512)
        for t in range(9):
            ps = R.get()
            for kc in range(KC):
                P.op("pe", lambda e, ps=ps, kc=kc, t=t, wb=wb: e.matmul(ps[:, :], hT[:, kc, t * 128:(t + 1) * 128], wb[:, kc, :],
                                                               start=(kc == 0), stop=(kc == KC - 1)),
                     reads=WStream.reads(wb) + hr, writes=[ps])
            tb = tvb[ti_ % 2]
            ti_ += 1
            P.op("act", lambda e, ps=ps, tb=tb: e.copy(out=tb[:], in_=ps[:]), reads=[ps], writes=[tb])
            P.dma("sp", rv_o[t * 128:(t + 1) * 128, blk * 512:(blk + 1) * 512], tb[:], reads=[tb])


def rope_tables():
    n = 32
    freq = 10000.0 ** (-np.arange(n, dtype=np.float32) / n)
    row = np.repeat(np.arange(SEQ // 64, dtype=np.float32), 64)
    col = (np.arange(SEQ) % 64).astype(np.float32)
    ang = np.concatenate([row[:, None] * freq, col[:, None] * freq], axis=-1)
    ang = np.concatenate([ang, ang], axis=-1).astype(np.float32)
    cos = np.cos(ang).astype(np.float32).T
    sin = np.sin(ang).astype(np.float32).T
    sin = sin.copy()
    sin[:64] *= -1.0
    perm = np.zeros((128, 128), np.float32)
    for m in range(64):
        perm[m + 64, m] = 1.0
        perm[m, m + 64] = 1.0
    return np.ascontiguousarray(cos), np.ascontiguousarray(sin), perm


def core_tokens_T(x_b, s_b, half):
    a = s_b[half * 128:(half + 1) * 128]
    b = x_b[half * 1024:(half + 1) * 1024]
    return np.ascontiguousarray(np.concatenate([a, b], axis=0).T)


def run_prog(P, in_maps):
    nc = P.finish() if isinstance(P, Prog) else P
    res = run_bass_kernel_spmd(nc, in_maps, core_ids=list(range(8)))
    return res.results


P2_STAGE = [99]


class _Stop(Exception):
    pass


def _chk(k):
    if P2_STAGE[0] == k:
        raise _Stop()


NS = 2304
NCH = 18
SEG = [(0, 256, 2), (256, 2048, 262)]
XPW = 2312


def p2_consts():
    i = np.arange(128)
    ident = np.eye(128, dtype=np.float32)
    triU = (i[:, None] <= i[None, :]).astype(np.float32)
    maskUs = (i[:, None] < i[None, :]).astype(np.float32)
    maskLi = (i[:, None] >= i[None, :]).astype(np.float32)
    maskLs = (i[:, None] > i[None, :]).astype(np.float32)
    dpos = np.maximum(i[None, :] - i[:, None], 0).astype(np.float32)
    ip1 = np.broadcast_to((i + 1).astype(np.float32)[None, :], (128, 128)).copy()
    cm = np.broadcast_to((127 - i).astype(np.float32)[:, None], (128, 128)).copy()
    return np.ascontiguousarray(np.concatenate([ident, triU, maskUs, maskLi, maskLs, dpos, ip1, cm], axis=1))


def build_p2():
    P = Prog()
    try:
        _build_p2(P)
    except _Stop:
        pass
    return P


def _build_p2(P):
    nc = P.nc
    qkv_d = nc.dram_tensor("qkvT", [1536, NS], F32, kind="ExternalInput").ap()
    cw_d = nc.dram_tensor("convw", [128, 60], F32, kind="ExternalInput").ap()
    a_d = nc.dram_tensor("a_tok", [128, 72], F32, kind="ExternalInput").ap()
    b_d = nc.dram_tensor("b_tok", [128, 72], F32, kind="ExternalInput").ap()
    hp_d = nc.dram_tensor("hp", [128, 12], F32, kind="ExternalInput").ap()
    rq_d = nc.dram_tensor("rqT", [512, NS], F32, kind="ExternalInput").ap()
    rk_d = nc.dram_tensor("rkT", [512, NS], F32, kind="ExternalInput").ap()
    rv_d = nc.dram_tensor("rv", [NS, 1024], F32, kind="ExternalInput").ap()
    cst_d = nc.dram_tensor("cst", [128, 8 * 128], F32, kind="ExternalInput").ap()
    og_o = nc.dram_tensor("ogT", [512, NS], F32, kind="ExternalOutput").ap()
    or_o = nc.dram_tensor("orT", [1024, NS], F32, kind="ExternalOutput").ap()

    R = PsumRing(P)
    cst = P.sb([128, 1024], F32, name="cst_sb")
    ident, triU, maskUs, maskLi, maskLs, dpos, ip1, cm = [cst[:, k * 128:(k + 1) * 128] for k in range(8)]
    maskUi = triU
    ones = P.sb([128, 128], F32, name="ones")
    cw = P.sb([128, 60], F32, name="cw")
    a_t = P.sb([128, 72], F32, name="a_t")
    b_t = P.sb([128, 72], F32, name="b_t")
    hp = P.sb([128, 12], F32, name="hp_sb")
    hq = P.sb([128, 12], F32, name="hq")
    P.i("pool", "memset", [], [ones], ones[:], 1.0)
    P.dma("sp", cst[:], cst_d, writes=[cst])
    P.dma("sp", cw[:], cw_d, writes=[cw])
    P.dma("sp", a_t[:], a_d, writes=[a_t])
    P.dma("sp", b_t[:], b_d, writes=[b_t])
    P.dma("sp", hp[:], hp_d, writes=[hp])
    P.i("act", "activation", [hp], [(hq, 0)], out=hq[:, 0:4], in_=hp[:, 0:4], func=AF.Exp)
    P.i("dve", "tensor_scalar", [(hq, 0)], [(hq, 0)], out=hq[:, 0:4], in0=hq[:, 0:4], scalar1=-1.0, scalar2=None, op0=ALU.mult)
    P.i("act", "activation", [hp], [(hq, 1)], out=hq[:, 4:8], in_=hp[:, 8:12], func=AF.Exp, scale=-1.0)
    P.i("act", "activation", [(hq, 1)], [(hq, 1)], out=hq[:, 4:8], in_=hq[:, 4:8], func=AF.Ln, bias=1.0)
    P.i("dve", "tensor_scalar", [(hq, 1)], [(hq, 1)], out=hq[:, 4:8], in0=hq[:, 4:8], scalar1=-1.0, scalar2=None, op0=ALU.mult)

    W = [P.sb([128, NCH, 128], F32, name=f"W{i}") for i in range(10)]
    xp = [P.sb([128, XPW], F32, name=f"xp{i}") for i in range(2)]
    for t in xp:
        P.i("pool", "memset", [], [t], t[:], 0.0)
    small = P.sb([128, 12, NCH], F32, name="small")
    sm = lambda k: small[:, k, :]
    tmpA = [P.sb([128, 128], F32, name=f"tmpA{i}") for i in range(4)]
    tmpB = [P.sb([128, 128], F32, name=f"tmpB{i}") for i in range(4)]
    tmpC = [P.sb([128, 256], F32, name=f"tmpC{i}") for i in range(4)]
    Sg = [P.sb([128, 128], F32, name=f"Sg{i}") for i in range(2)]
    Sr = [P.sb([128, 256], F32, name=f"Sr{i}") for i in range(2)]
    rvt = P.sb([128, NCH, 256], F32, name="rvt")
    orT = P.sb([128, 2, NS], F32, name="orT_sb")
    rc = [P.sb([128, 128], F32, name=f"rc{i}") for i in range(3)]
    rcol = P.sb([128, 4], F32, name="rcol")
    cnt = [0]

    def rot(lst):
        cnt[0] += 1
        return lst[cnt[0] % len(lst)]

    def flat(Wt):
        return Wt[:].rearrange("p a b -> p (a b)")

    def allk(Wt):
        return [(Wt, n) for n in range(NCH)]

    xpi = [0]

    def preprocess(chunk, dst, normalize, qscale):
        x = xp[xpi[0] % 2]
        xpi[0] += 1
        for (o, n, po) in SEG:
            P.dma("sp", x[:, po:po + n], qkv_d[chunk * 128:(chunk + 1) * 128, o:o + n], writes=[x])
        d = flat(dst)
        for si, (o, n, po) in enumerate(SEG):
            eng = "dve"
            P.i("act", "activation", [x, cw], allk(dst), out=d[:, o:o + n], in_=x[:, po - 2:po - 2 + n], func=AF.Identity,
                scale=cw[:, chunk * 5:chunk * 5 + 1])
            for j in range(1, 5):
                P.i(eng, "scalar_tensor_tensor", [x, cw] + allk(dst), allk(dst), out=d[:, o:o + n], in0=x[:, po - 2 + j:po - 2 + j + n],
                    scalar=cw[:, chunk * 5 + j:chunk * 5 + j + 1], in1=d[:, o:o + n], op0=ALU.mult, op1=ALU.add)
        P.i("act", "activation", allk(dst), allk(dst), out=d[:, :], in_=d[:, :], func=AF.Silu)
        if normalize:
            sq = flat(W[9])
            P.i("act", "activation", allk(dst), allk(W[9]), out=sq[:, :], in_=d[:, :], func=AF.Square)
            for o in range(0, NS, 512):
                n = min(512, NS - o)
                ps = R.get()
                P.mm(ps[:, :n], ones[:], sq[:, o:o + n], True, True, allk(W[9]) + [ones], [ps])
                P.i("act", "activation", [ps], allk(W[9]), out=sq[:, o:o + n], in_=ps[:, :n], func=AF.Sqrt, bias=EPS, scale=1.0)
            P.i("dve", "reciprocal", allk(W[9]), allk(W[9]), out=sq[:, :], in_=sq[:, :])
            P.i("dve", "scalar_tensor_tensor", allk(W[9]) + allk(dst), allk(dst), out=d[:, :], in0=d[:, :], scalar=qscale, in1=sq[:, :],
                op0=ALU.mult, op1=ALU.mult)

    def transpose_chunks(src, dst, scale_col=None):
        for n0 in range(0, NCH, 4):
            n1 = min(NCH, n0 + 4)
            ps = R.get()
            for n in range(n0, n1):
                P.op("pe", lambda e, n=n, ps=ps, n0=n0: e.transpose(out=ps[:, (n - n0) * 128:(n - n0 + 1) * 128], in_=src[:, n, :], identity=ident),
                     reads=[(src, n), cst], writes=[ps])
            w = (n1 - n0) * 128
            if scale_col is None:
                P.i("act", "copy", [ps], [(dst, n) for n in range(n0, n1)], out=flat(dst)[:, n0 * 128:n0 * 128 + w], in_=ps[:, :w])
            else:
                P.i("dve", "tensor_scalar", [ps, rcol], [(dst, n) for n in range(n0, n1)], out=flat(dst)[:, n0 * 128:n0 * 128 + w], in0=ps[:, :w],
                    scalar1=scale_col, scalar2=None, op0=ALU.mult)

    QT, KT, VT, Ktok, Vtok, Rall, Pw, PTw, TT, nwT = W
    for h in range(4):
        preprocess(h, QT, True, 128.0 ** -0.5)
        preprocess(4 + h, KT, True, 1.0)
        preprocess(8 + h, VT, False, 1.0)
        _chk(1)
        transpose_chunks(KT, Ktok)
        transpose_chunks(VT, Vtok)
        _chk(2)
        oT = VT
        g, beta, gc, gl, egc, bege, ekd, egl = [sm(k) for k in range(8)]
        sl = slice(h * NCH, (h + 1) * NCH)
        P.i("act", "activation", [a_t, hp], [(small, 0)], out=g, in_=a_t[:, sl], func=AF.Exp, bias=hp[:, 4 + h:5 + h], scale=1.0)
        P.i("act", "activation", [(small, 0)], [(small, 0)], out=g, in_=g, func=AF.Ln, bias=1.0)
        P.i("dve", "tensor_scalar", [(small, 0), (hq, 0)], [(small, 0)], out=g, in0=g, scalar1=hq[:, h:h + 1], scalar2=None, op0=ALU.mult)
        P.i("act", "activation", [b_t], [(small, 1)], out=beta, in_=b_t[:, sl], func=AF.Sigmoid)
        ps = R.get()
        P.mm(ps[:, 0:NCH], triU, g, True, True, [(small, 0), cst], [ps])
        P.mm(ps[:, 32:32 + NCH], ones[:], g, True, True, [(small, 0), ones], [ps])
        P.i("dve", "tensor_copy", [ps], [(small, 2)], out=gc, in_=ps[:, 0:NCH])
        P.i("dve", "tensor_copy", [ps], [(small, 3)], out=gl, in_=ps[:, 32:32 + NCH])
        P.i("act", "activation", [(small, 2)], [(small, 4)], out=egc, in_=gc, func=AF.Exp)
        P.i("dve", "tensor_tensor", [(small, 4), (small, 1)], [(small, 5)], out=bege, in0=egc, in1=beta, op=ALU.mult)
        P.i("dve", "tensor_tensor", [(small, 3), (small, 2)], [(small, 6)], out=ekd, in0=gl, in1=gc, op=ALU.subtract)
        P.i("act", "activation", [(small, 6)], [(small, 6)], out=ekd, in_=ekd, func=AF.Exp)
        P.i("act", "activation", [(small, 3)], [(small, 7)], out=egl, in_=gl, func=AF.Exp)
        _chk(3)
        for n in range(NCH):
            G = rot(tmpA)
            P.i("dve", "tensor_scalar", [cst, (small, 0)], [G], out=G[:], in0=triU, scalar1=g[:, n:n + 1], scalar2=None, op0=ALU.mult)
            ps = R.get()
            P.mm(ps[:, 0:128], ones[:], G[:], True, True, [G, ones], [ps])
            P.mm(ps[:, 128:256], KT[:, n, :], KT[:, n, :], True, True, [(KT, n)], [ps])
            P.i("act", "copy", [ps], [(Rall, n)], out=Rall[:, n, :], in_=ps[:, 0:128])
            _chk(31)
            t = rot(tmpB)
            P.i("dve", "tensor_scalar", [(Rall, n), (small, 2)], [t], out=t[:], in0=Rall[:, n, :], scalar1=gc[:, n:n + 1], scalar2=-1.0,
                op0=ALU.subtract, op1=ALU.mult)
            P.i("pool", "tensor_tensor", [t, cst], [t], out=t[:], in0=t[:], in1=maskLi, op=ALU.mult)
            P.i("act", "activation", [t], [t], out=t[:], in_=t[:], func=AF.Exp)
            P.i("pool", "tensor_tensor", [t, cst], [t], out=t[:], in0=t[:], in1=maskLs, op=ALU.mult)
            _chk(32)
            P.i("dve", "scalar_tensor_tensor", [ps, (small, 1), t], [(Pw, n)], out=Pw[:, n, :], in0=ps[:, 128:256], scalar=beta[:, n:n + 1],
                in1=t[:], op0=ALU.mult, op1=ALU.mult)
            _chk(33)
            ps2 = R.get()
            P.op("pe", lambda e, n=n, ps2=ps2: e.transpose(out=ps2[:, 0:128], in_=Pw[:, n, :], identity=ident), reads=[(Pw, n), cst], writes=[ps2])
            P.i("act", "copy", [ps2], [(PTw, n)], out=PTw[:, n, :], in_=ps2[:, 0:128])
            P.i("dve", "scalar_tensor_tensor", [(PTw, n), cst], [(TT, n)], out=TT[:, n, :], in0=PTw[:, n, :], scalar=-1.0, in1=ident,
                op0=ALU.mult, op1=ALU.add)
        _chk(4)
        for m in range(1, 7):
            last = (m == 6)
            for n0 in range(0, NCH, 4):
                n1 = min(NCH, n0 + 4)
                w = (n1 - n0) * 128
                rng = list(range(n0, n1))
                psP = R.get()
                psT = None if last else R.get()
                for n in rng:
                    o = (n - n0) * 128
                    P.mm(psP[:, o:o + 128], PTw[:, n, :], Pw[:, n, :], True, True, [(PTw, n), (Pw, n)], [psP])
                    if not last:
                        P.mm(psT[:, o:o + 128], Pw[:, n, :], PTw[:, n, :], True, True, [(PTw, n), (Pw, n)], [psT])
                P.i("act", "copy", [psP], [(Pw, n) for n in rng], out=flat(Pw)[:, n0 * 128:n0 * 128 + w], in_=psP[:, :w])
                if not last:
                    P.i("dve", "tensor_copy", [psT], [(PTw, n) for n in rng], out=flat(PTw)[:, n0 * 128:n0 * 128 + w], in_=psT[:, :w])
                psU = R.get()
                for n in rng:
                    o = (n - n0) * 128
                    P.mm(psU[:, o:o + 128], Pw[:, n, :], TT[:, n, :], True, True, [(Pw, n), (TT, n)], [psU])
                P.i("dve", "tensor_tensor", [psU] + [(TT, n) for n in rng], [(TT, n) for n in rng], out=flat(TT)[:, n0 * 128:n0 * 128 + w],
                    in0=flat(TT)[:, n0 * 128:n0 * 128 + w], in1=psU[:, :w], op=ALU.add)
        _chk(5)
        QKT, QgT = Pw, PTw
        for n in range(NCH):
            kbe = rot(tmpA)
            P.i("pool", "tensor_scalar", [(Ktok, n), (small, 5)], [kbe], out=kbe[:], in0=Ktok[:, n, :], scalar1=bege[:, n:n + 1], scalar2=None, op0=ALU.mult)
            ps = R.get()
            P.mm(ps[:, 0:128], kbe[:], TT[:, n, :], True, True, [kbe, (TT, n)], [ps])
            P.i("act", "mul", [ps], [(nwT, n)], out=nwT[:, n, :], in_=ps[:, 0:128], mul=-1.0)
            psq = R.get()
            P.mm(psq[:, 128:256], KT[:, n, :], QT[:, n, :], True, True, [(KT, n), (QT, n)], [psq])
            t = rot(tmpB)
            P.i("dve", "tensor_scalar", [(Rall, n), (small, 2)], [t], out=t[:], in0=Rall[:, n, :], scalar1=gc[:, n:n + 1], scalar2=None, op0=ALU.subtract)
            P.i("pool", "tensor_tensor", [t, cst], [t], out=t[:], in0=t[:], in1=maskUi, op=ALU.mult)
            P.i("act", "activation", [t], [t], out=t[:], in_=t[:], func=AF.Exp)
            P.i("pool", "tensor_tensor", [t, cst], [t], out=t[:], in0=t[:], in1=maskUi, op=ALU.mult)
            P.i("dve", "tensor_tensor", [psq, t], [(QKT, n)], out=QKT[:, n, :], in0=psq[:, 128:256], in1=t[:], op=ALU.mult)
            e_ = rot(tmpB)
            P.i("act", "activation", [(Rall, n)], [e_], out=e_[:], in_=Rall[:, n, :], func=AF.Exp)
            P.i("pool", "tensor_tensor", [e_, (QT, n)], [(QgT, n)], out=QgT[:, n, :], in0=QT[:, n, :], in1=e_[:], op=ALU.mult)
        _chk(6)
        P.i("pool", "memset", [], [Sg[0]], Sg[0][:], 0.0)
        for n in range(NCH):
            S, S2 = Sg[n % 2], Sg[(n + 1) % 2]
            vb = rot(tmpA)
            P.i("pool", "tensor_scalar", [(Vtok, n), (small, 1)], [vb], out=vb[:], in0=Vtok[:, n, :], scalar1=beta[:, n:n + 1], scalar2=None, op0=ALU.mult)
            kd = rot(tmpA)
            P.i("pool", "tensor_scalar", [(Ktok, n), (small, 6)], [kd], out=kd[:], in0=Ktok[:, n, :], scalar1=ekd[:, n:n + 1], scalar2=None, op0=ALU.mult)
            ps = R.get()
            P.mm(ps[:, 0:128], TT[:, n, :], vb[:], True, False, [(TT, n), vb], [ps])
            P.mm(ps[:, 0:128], nwT[:, n, :], S[:], False, True, [(nwT, n), S], [ps])
            vnew = rot(tmpB)
            P.i("act", "copy", [ps], [vnew], out=vnew[:], in_=ps[:, 0:128])
            ps2 = R.get()
            P.mm(ps2[:, 0:128], S[:], QgT[:, n, :], True, False, [S, (QgT, n)], [ps2])
            P.mm(ps2[:, 0:128], vnew[:], QKT[:, n, :], False, True, [vnew, (QKT, n)], [ps2])
            P.i("act", "copy", [ps2], [(oT, n)], out=oT[:, n, :], in_=ps2[:, 0:128])
            ps3 = R.get()
            P.mm(ps3[:, 0:128], kd[:], vnew[:], True, True, [kd, vnew], [ps3])
            P.i("dve", "scalar_tensor_tensor", [S, (small, 7), ps3], [S2], out=S2[:], in0=S[:], scalar=egl[:, n:n + 1], in1=ps3[:, 0:128],
                op0=ALU.mult, op1=ALU.add)
        P.dma("sp", og_o[h * 128:(h + 1) * 128, :], flat(oT), reads=allk(oT))
        _chk(7)

    RQ, RK, RKtok, AT, Qd = W[0], W[1], W[2], W[3], W[4]
    for h in range(4):
        lg = hq[:, 4 + h:5 + h]
        P.dma("sp", flat(RQ), rq_d[h * 128:(h + 1) * 128, :], writes=allk(RQ))
        P.dma("sp", flat(RK), rk_d[h * 128:(h + 1) * 128, :], writes=allk(RK))
        rvsrc = rv_d[:, h * 256:(h + 1) * 256].rearrange("(n p) d -> p n d", p=128)
        for n0 in range(0, NCH, 3):
            P.dma("sp", rvt[:, n0:n0 + 3, :], rvsrc[:, n0:n0 + 3, :], writes=[rvt])
        decT, cd = rc[0], rc[1]
        P.i("act", "activation", [cst, (hq, 1)], [decT], out=decT[:], in_=dpos, func=AF.Exp, scale=lg)
        P.i("pool", "tensor_tensor", [decT, cst], [decT], out=decT[:], in0=decT[:], in1=maskUi, op=ALU.mult)
        P.i("act", "activation", [cst, (hq, 1)], [cd], out=cd[:], in_=ip1, func=AF.Exp, scale=lg)
        P.i("act", "activation", [cst, (hq, 1)], [rcol], out=rcol[:, 0:1], in_=cm[:, 0:1], func=AF.Exp, scale=lg)
        P.i("act", "activation", [cst, (hq, 1)], [rcol], out=rcol[:, 1:2], in_=ip1[:, 127:128], func=AF.Exp, scale=lg)
        transpose_chunks(RK, RKtok, scale_col=rcol[:, 0:1])
        P.i("pool", "memset", [], [Sr[0]], Sr[0][:], 0.0)
        for n in range(NCH):
            S, S2 = Sr[n % 2], Sr[(n + 1) % 2]
            ps = R.get()
            P.mm(ps[:, 0:128], RK[:, n, :], RQ[:, n, :], True, True, [(RK, n), (RQ, n)], [ps])
            at = rot(tmpA)
            P.i("dve", "tensor_tensor", [ps, decT], [at], out=at[:], in0=ps[:, 0:128], in1=decT[:], op=ALU.mult)
            qd = rot(tmpB)
            P.i("pool", "tensor_tensor", [(RQ, n), cd], [qd], out=qd[:], in0=RQ[:, n, :], in1=cd[:], op=ALU.mult)
            ps2 = R.get()
            for a in range(2):
                P.mm(ps2[:, a * 128:(a + 1) * 128], rvt[:, n, a * 128:(a + 1) * 128], at[:], True, False, [rvt, at], [ps2])
                P.mm(ps2[:, a * 128:(a + 1) * 128], S[:, a * 128:(a + 1) * 128], qd[:], False, True, [S, qd], [ps2])
            for a in range(2):
                P.i("act", "copy", [ps2], [(orT, a, n)], out=orT[:, a, n * 128:(n + 1) * 128], in_=ps2[:, a * 128:(a + 1) * 128])
            ps3 = R.get()
            P.mm(ps3[:, 0:256], RKtok[:, n, :], rvt[:, n, :], True, True, [(RKtok, n), rvt], [ps3])
            P.i("dve", "scalar_tensor_tensor", [S, rcol, ps3], [S2], out=S2[:], in0=S[:], scalar=rcol[:, 1:2], in1=ps3[:, 0:256],
                op0=ALU.mult, op1=ALU.add)
        for a in range(2):
            P.dma("sp", or_o[h * 256 + a * 128:h * 256 + (a + 1) * 128, :], orT[:, a, :], reads=[(orT, a, n) for n in range(NCH)])
    return P


def seq_from_halves(c0, c1, axis_tok, flip):
    a0 = np.moveaxis(c0, axis_tok, 0)
    a1 = np.moveaxis(c1, axis_tok, 0)
    ctx = np.concatenate([a0[:128], a1[:128]], axis=0)
    lat = np.concatenate([a0[128:], a1[128:]], axis=0)
    if flip:
        ctx = ctx[::-1]
        lat = lat[::-1]
    return np.ascontiguousarray(np.moveaxis(np.concatenate([ctx, lat], axis=0), 0, axis_tok))


def halves_from_seq(full, axis_tok, flip):
    a = np.moveaxis(full, axis_tok, 0)
    ctx, lat = a[:256], a[256:]
    if flip:
        ctx = ctx[::-1]
        lat = lat[::-1]
    outs = []
    for half in range(2):
        o = np.concatenate([ctx[half * 128:(half + 1) * 128], lat[half * 1024:(half + 1) * 1024]], axis=0)
        outs.append(np.ascontiguousarray(np.moveaxis(o, 0, axis_tok)))
    return outs


def prep_p2(p1res, l, inp, cst):
    in_maps = []
    for core in range(8):
        b, d = core // 2, core % 2
        r0, r1 = p1res[2 * b], p1res[2 * b + 1]
        fl = (d == 1)
        ab = seq_from_halves(r0["abT"], r1["abT"], 1, fl)
        a_tok = np.concatenate([ab[d * 4 + h].reshape(NCH, 128).T for h in range(4)], axis=1)
        b_tok = np.concatenate([ab[8 + d * 4 + h].reshape(NCH, 128).T for h in range(4)], axis=1)
        cwf = inp["gdn_conv_w"][l]
        if fl:
            cwf = cwf[::-1]
        convw = np.ascontiguousarray(cwf.T.reshape(12, 128, 5).transpose(1, 0, 2).reshape(128, 60))
        hp = np.concatenate([inp["gdn_a_log"][l, d], inp["gdn_dt_bias"][l, d], inp["ret_decay_logit"][l, d]])[None, :]
        hp = np.ascontiguousarray(np.broadcast_to(hp, (128, 12))).astype(np.float32)
        in_maps.append(dict(
            qkvT=seq_from_halves(r0["qkvT"], r1["qkvT"], 1, fl),
            convw=convw.astype(np.float32), a_tok=np.ascontiguousarray(a_tok), b_tok=np.ascontiguousarray(b_tok), hp=hp,
            rqT=seq_from_halves(r0["rqT"], r1["rqT"], 1, fl), rkT=seq_from_halves(r0["rkT"], r1["rkT"], 1, fl),
            rv=seq_from_halves(r0["rv"], r1["rv"], 0, fl), cst=cst))
    return in_maps


GN = 384
NV = 15
P34_STAGE = [99]


class WPool:
    def __init__(self, P, n=4):
        self.P = P
        self.bufs = [P.sb([128, 8192], BF16, name=f"wp{i}") for i in range(n)]
        self.i = 0

    def load(self, w_ap, c0, ncols, kchunks):
        b = self.bufs[self.i % len(self.bufs)]
        self.i += 1
        v = b[:, 0:kchunks * ncols].rearrange("p (k n) -> p k n", n=ncols)
        src = w_ap[:, c0:c0 + ncols].rearrange("(kc p) n -> p kc n", p=128)
        step = max(1, 2048 // ncols)
        for k0 in range(0, kchunks, step):
            k1 = min(kchunks, k0 + step)
            self.P.dma("pool", v[:, k0:k1, :], src[:, k0:k1, :], writes=[b])
        return b, v


def build_p34(final=False, nv=8):
    P = Prog()
    nc = P.nc
    din = lambda name, shape: nc.dram_tensor(name, list(shape), F32, kind="ExternalInput").ap()
    xT_a = din("xT", [nv, D, NT])
    vec_a = din("vec", [nv, 128, NV * 16])
    wuv_d = din("w_uv", [D, 1024])
    wpost_d = din("w_post", [D, 7680])
    og_a = din("og", [nv, 2, 512, NT])
    or_a = din("orr", [nv, 2, 1024, NT])
    gnw_d = din("gnw", [128, 512])
    wsT_d = din("wsT", [128, 512])
    bsb_d = din("bsb", [128, 512])
    nrm_d = din("nrm", [128, 9])
    wgm_d = din("wbr_gm", [512, D])
    wgd_d = din("wbr_gdn", [512, D])
    wrt_d = din("wbr_ret", [1024, D])
    wout_d = din("w_out", [D, D])
    wr_d = din("wr", [D, 36])
    rb_d = din("rb", [128, 36])
    mg_d = din("mg", [32, D, 512])
    mu_d = din("mu", [32, D, 512])
    md_d = din("md", [32, 512, D])
    id_d = din("ident", [128, 128])
    xo_a = nc.dram_tensor("xo", [nv, D, NT], F32, kind="ExternalOutput").ap()
    cur = {}

    R = PsumRing(P)
    WP = WPool(P, 4)
    vec = P.sb([128, NV * 16], F32, name="vec_sb")
    ones = P.sb([128, 128], F32, name="ones")
    ident = P.sb([128, 128], F32, name="ident_sb")
    gnw = P.sb([128, 512], F32, name="gnw_sb")
    wsT = P.sb([128, 512], BF16, name="wsT_sb")
    bsb = P.sb([128, 512], F32, name="bsb_sb")
    nrm = P.sb([128, 9], F32, name="nrm_sb")
    wr = P.sb([128, KC, 36], F32, name="wr_sb")
    rb = P.sb([128, 36], F32, name="rb_sb")
    scl = P.sb([128, 64], F32, name="scl")
    P.i("pool", "memset", [], [ones], ones[:], 1.0)
    for t, d_ in ((ident, id_d), (gnw, gnw_d), (bsb, bsb_d), (nrm, nrm_d), (rb, rb_d)):
        P.dma("sp", t[:], d_, writes=[t])
    P.dma("pool", wsT[:], wsT_d, writes=[wsT])
    P.dma("sp", wr[:], wr_d.rearrange("(kc p) n -> p kc n", p=128), writes=[wr])
    V = lambda i, kc=None: vec[:, i * 16:(i + 1) * 16] if kc is None else vec[:, i * 16 + kc:i * 16 + kc + 1]
    def setup_v(v):
        cur["xT"], cur["og"], cur["or"], cur["xo"] = xT_a[v], og_a[v], or_a[v], xo_a[v]
        P.dma("sp", vec[:], vec_a[v], writes=[vec])
        for j, (iw, isc) in enumerate(((0, 1), (0, 3), (7, 8), (7, 10))):
            P.i("dve", "scalar_tensor_tensor", [vec], [(scl, j)], out=scl[:, j * 16:(j + 1) * 16], in0=V(isc), scalar=1.0, in1=V(iw),
                op0=ALU.add, op1=ALU.mult)

    xg = P.sb([128, KC, GN], F32, name="xg")
    hb = P.sb([128, KC, GN], BF16, name="hb")
    h2f = P.sb([128, KC, GN], F32, name="h2f")
    scr = [P.sb([128, GN], F32, name=f"scr{i}") for i in range(3)]
    rstd = P.sb([128, GN], F32, name="rstd")
    vn = P.sb([128, 3, 512], BF16, name="vn")
    vg = [P.sb([128, 512], F32, name=f"vg{i}") for i in range(2)]
    st = P.sb([128, 8], F32, name="st")
    uT = P.sb([128, 4, GN], F32, name="uT")
    gmT = P.sb([128, 4, GN], BF16, name="gmT")
    ognT = P.sb([128, 4, GN], BF16, name="ognT")
    ornT = P.sb([128, 8, GN], BF16, name="ornT")
    zT = P.sb([128, KC, GN], BF16, name="zT")
    og2 = [P.sb([128, GN], F32, name=f"og2_{i}") for i in range(4)]
    gate = [P.sb([128, GN], F32, name=f"gate{i}") for i in range(2)]
    sg = [P.sb([128, GN], F32, name=f"sg{i}") for i in range(3)]
    zacc = [P.sb([128, GN], F32, name=f"zacc{i}") for i in range(2)]
    actT = P.sb([128, 4, GN], BF16, name="actT")
    Gb = [P.sb([128, GN], F32, name=f"Gb{i}") for i in range(2)]
    GT = P.sb([32, GN], F32, name="GT")
    GTm = [P.sb([32, GN], F32, name=f"GTm{i}") for i in range(2)]
    rt = P.sb([128, 8, 40], F32, name="rt")
    cnt = [0]

    def rot(lst):
        cnt[0] += 1
        return lst[cnt[0] % len(lst)]

    def hreads(t):
        return [(t, kc) for kc in range(KC)]

    def norm(rngs, sclbase, ish_l, ish_c, out_f32, out_bf):
        ps = R.get()
        for kc in range(KC):
            s_ = rot(scr)
            P.i("act", "activation", [(xg, kc)], [s_], out=s_[:], in_=xg[:, kc, :], func=AF.Square)
            P.mm(ps[:, :GN], ones[:], s_[:], kc == 0, kc == KC - 1, [s_, ones], [ps])
        P.i("act", "activation", [ps], [rstd], out=rstd[:], in_=ps[:, :GN], func=AF.Sqrt, bias=EPS, scale=1.0 / D)
        P.i("dve", "reciprocal", [rstd], [rstd], out=rstd[:], in_=rstd[:])
        for kc in range(KC):
            s_ = rot(scr)
            P.i("dve", "tensor_tensor", [(xg, kc), rstd], [s_], out=s_[:], in0=xg[:, kc, :], in1=rstd[:], op=ALU.mult)
            dst = out_f32 if out_f32 is not None else out_bf
            for (o, n, isc) in rngs:
                sc = scl[:, sclbase + (16 if isc else 0) + kc:sclbase + (16 if isc else 0) + kc + 1]
                P.i("act", "activation", [s_, vec, (scl, 0), (scl, 1), (scl, 2), (scl, 3)], [(dst, kc)], out=dst[:, kc, o:o + n], in_=s_[:, o:o + n],
                    func=AF.Identity, bias=V(ish_c if isc else ish_l, kc), scale=sc)
            if out_f32 is not None:
                P.i("pool", "tensor_copy", [(out_f32, kc)], [(out_bf, kc)], out=out_bf[:, kc, :], in_=out_f32[:, kc, :])

    def proj(wv, wb, cc, hT, kchunks=KC, width=128):
        ps = R.get()
        for kc in range(kchunks):
            P.mm(ps[:width, :GN], wv[:, kc, cc * 128:cc * 128 + width], hT[:, kc, :], kc == 0, kc == kchunks - 1,
                 [wb] + [(hT, k) for k in range(kchunks)], [ps])
        return ps

    def group(gi):
        xT_d, og_d, or_d = cur["xT"], cur["og"], cur["or"]
        c0 = gi * GN
        rngs = [(0, 128, True), (128, 256, False)] if gi == 0 else [(0, GN, False)]
        for kc in range(KC):
            P.dma("sp", xg[:, kc, :], xT_d[kc * 128:(kc + 1) * 128, c0:c0 + GN], writes=[(xg, kc)])
        norm(rngs, 0, 2, 4, None, hb)
        wb, wv = WP.load(wuv_d, 512, 512, KC)
        for t in range(3):
            ps = R.get()
            for kc in range(KC):
                P.mm(ps[:, :], hb[:, kc, t * 128:(t + 1) * 128], wv[:, kc, :], kc == 0, kc == KC - 1, [wb] + hreads(hb), [ps])
            g_ = rot(vg)
            P.i("act", "activation", [ps], [g_], out=g_[:], in_=ps[:, :], func=AF.Gelu)
            P.i("dve", "reduce_sum", [g_], [(st, 0)], out=st[:, 0:1], in_=g_[:], axis=AX.X)
            P.i("dve", "tensor_scalar", [(st, 0)], [(st, 0)], out=st[:, 0:1], in0=st[:, 0:1], scalar1=1.0 / 512, scalar2=None, op0=ALU.mult)
            P.i("dve", "tensor_scalar", [g_, (st, 0)], [g_], out=g_[:], in0=g_[:], scalar1=st[:, 0:1], scalar2=None, op0=ALU.subtract)
            q_ = rot(vg)
            P.i("dve", "tensor_tensor", [g_], [q_], out=q_[:], in0=g_[:], in1=g_[:], op=ALU.mult)
            P.i("dve", "reduce_sum", [q_], [(st, 1)], out=st[:, 1:2], in_=q_[:], axis=AX.X)
            P.i("act", "activation", [(st, 1)], [(st, 1)], out=st[:, 1:2], in_=st[:, 1:2], func=AF.Sqrt, bias=EPS, scale=1.0 / 512)
            P.i("dve", "reciprocal", [(st, 1)], [(st, 1)], out=st[:, 1:2], in_=st[:, 1:2])
            P.i("dve", "scalar_tensor_tensor", [g_, (st, 1), gnw], [(vn, t)], out=vn[:, t, :], in0=g_[:], scalar=st[:, 1:2], in1=gnw[:],
                op0=ALU.mult, op1=ALU.mult)
        wb, wv = WP.load(wuv_d, 0, 512, KC)
        for cc in range(4):
            ps = proj(wv, wb, cc, hb)
            P.i("act", "activation", [ps], [(uT, cc)], out=uT[:, cc, :], in_=ps[:, :GN], func=AF.Gelu)
        for g4 in range(4):
            ps = R.get()
            for t in range(3):
                P.mm(ps[:, t * 128:(t + 1) * 128], vn[:, t, g4 * 128:(g4 + 1) * 128], wsT[:, g4 * 128:(g4 + 1) * 128], True, True,
                     [(vn, t), wsT], [ps])
            s_ = rot(scr)
            for t in range(3):
                P.i("dve", "tensor_tensor", [ps, bsb], [s_], out=s_[:, t * 128:(t + 1) * 128], in0=ps[:, t * 128:(t + 1) * 128],
                    in1=bsb[:, g4 * 128:(g4 + 1) * 128], op=ALU.add)
            P.i("pool", "tensor_tensor", [s_, (uT, g4)], [(gmT, g4)], out=gmT[:, g4, :], in0=s_[:], in1=uT[:, g4, :], op=ALU.mult)
        if P34_STAGE[0] == 1:
            raise _Stop()
        wb, wv = WP.load(wpost_d, 0, 512, KC)
        for h in range(4):
            o2 = rot(og2)
            o3 = rot(og2)
            P.dma("sp", o2[:], og_d[0, h * 128:(h + 1) * 128, c0:c0 + GN], writes=[o2])
            P.dma("sp", o3[:], og_d[1, h * 128:(h + 1) * 128, c0:c0 + GN], writes=[o3])
            P.i("pool", "tensor_tensor", [o2, o3], [o2], out=o2[:], in0=o2[:], in1=o3[:], op=ALU.add)
            s_ = rot(scr)
            P.i("act", "activation", [o2], [s_], out=s_[:], in_=o2[:], func=AF.Square)
            ps = R.get()
            P.mm(ps[:, :GN], ones[:], s_[:], True, True, [s_, ones], [ps])
            P.i("act", "activation", [ps], [s_], out=s_[:], in_=ps[:, :GN], func=AF.Sqrt, bias=EPS, scale=1.0 / 128)
            P.i("dve", "reciprocal", [s_], [s_], out=s_[:], in_=s_[:])
            P.i("dve", "scalar_tensor_tensor", [o2, nrm, s_], [o2], out=o2[:], in0=o2[:], scalar=nrm[:, 0:1], in1=s_[:], op0=ALU.mult, op1=ALU.mult)
            psg = proj(wv, wb, h, hb)
            g_ = rot(gate)
            P.i("act", "activation", [psg], [g_], out=g_[:], in_=psg[:, :GN], func=AF.Silu)
            P.i("pool", "tensor_tensor", [o2, g_], [(ognT, h)], out=ognT[:, h, :], in0=o2[:], in1=g_[:], op=ALU.mult)
        for blk in range(2):
            wb, wv = WP.load(wpost_d, 512 + blk * 512, 512, KC)
            for hh in range(2):
                h = blk * 2 + hh
                oo = []
                for a in range(2):
                    o2 = rot(og2)
                    o3 = rot(og2)
                    r0 = h * 256 + a * 128
                    P.dma("sp", o2[:], or_d[0, r0:r0 + 128, c0:c0 + GN], writes=[o2])
                    P.dma("sp", o3[:], or_d[1, r0:r0 + 128, c0:c0 + GN], writes=[o3])
                    P.i("pool", "tensor_tensor", [o2, o3], [o2], out=o2[:], in0=o2[:], in1=o3[:], op=ALU.add)
                    oo.append(o2)
                    if a == 0:
                        pass
                ps = R.get()
                P.mm(ps[:, :GN], ones[:], oo[0][:], True, False, [oo[0], ones], [ps])
                P.mm(ps[:, :GN], ones[:], oo[1][:], False, True, [oo[1], ones], [ps])
                mean = rot(scr)
                P.i("act", "mul", [ps], [mean], out=mean[:], in_=ps[:, :GN], mul=1.0 / 256)
                for a in range(2):
                    P.i("dve", "tensor_tensor", [oo[a], mean], [oo[a]], out=oo[a][:], in0=oo[a][:], in1=mean[:], op=ALU.subtract)
                ps = R.get()
                for a in range(2):
                    s_ = rot(scr)
                    P.i("act", "activation", [oo[a]], [s_], out=s_[:], in_=oo[a][:], func=AF.Square)
                    P.mm(ps[:, :GN], ones[:], s_[:], a == 0, a == 1, [s_, ones], [ps])
                rs = rot(scr)
                P.i("act", "activation", [ps], [rs], out=rs[:], in_=ps[:, :GN], func=AF.Sqrt, bias=EPS, scale=1.0 / 256)
                P.i("dve", "reciprocal", [rs], [rs], out=rs[:], in_=rs[:])
                for a in range(2):
                    ci = h * 2 + a
                    P.i("dve", "scalar_tensor_tensor", [oo[a], nrm, rs], [oo[a]], out=oo[a][:], in0=oo[a][:], scalar=nrm[:, 1 + ci:2 + ci], in1=rs[:],
                        op0=ALU.mult, op1=ALU.mult)
                    psg = proj(wv, wb, hh * 2 + a, hb)
                    g_ = rot(gate)
                    P.i("act", "activation", [psg], [g_], out=g_[:], in_=psg[:, :GN], func=AF.Silu)
                    P.i("pool", "tensor_tensor", [oo[a], g_], [(ornT, ci)], out=ornT[:, ci, :], in0=oo[a][:], in1=g_[:], op=ALU.mult)
        if P34_STAGE[0] == 2:
            raise _Stop()
        for cb in range(4):
            lw = [WP.load(wpost_d, 1536 + k * 2048 + cb * 512, 512, KC) for k in range(2)]
            bgm = WP.load(wgm_d, cb * 512, 512, 4)
            bgd = WP.load(wgd_d, cb * 512, 512, 4)
            for cc in range(4):
                c = cb * 4 + cc
                sgs = []
                for k in range(2):
                    ps = proj(lw[k][1], lw[k][0], cc, hb)
                    s_ = sg[k]
                    P.i("act", "activation", [ps], [s_], out=s_[:], in_=ps[:, :GN], func=AF.Sigmoid)
                    sgs.append(s_)
                za = rot(zacc)
                ps = proj(bgm[1], bgm[0], cc, gmT, kchunks=4)
                P.i("dve", "tensor_tensor", [ps, sgs[0]], [za], out=za[:], in0=ps[:, :GN], in1=sgs[0][:], op=ALU.mult)
                ps = proj(bgd[1], bgd[0], cc, ognT, kchunks=4)
                t_ = rot(scr)
                P.i("dve", "tensor_tensor", [ps, sgs[1]], [t_], out=t_[:], in0=ps[:, :GN], in1=sgs[1][:], op=ALU.mult)
                P.i("pool", "tensor_tensor", [za, t_], [za], out=za[:], in0=za[:], in1=t_[:], op=ALU.add)
                P.i("pool", "tensor_copy", [za], [(h2f, c)], out=h2f[:, c, :], in_=za[:])
        for cb in range(4):
            brt = WP.load(wrt_d, cb * 512, 512, 8)
            lw2 = WP.load(wpost_d, 1536 + 2 * 2048 + cb * 512, 512, KC)
            for cc in range(4):
                c = cb * 4 + cc
                ps = proj(lw2[1], lw2[0], cc, hb)
                s_ = sg[2]
                P.i("act", "activation", [ps], [s_], out=s_[:], in_=ps[:, :GN], func=AF.Sigmoid)
                ps = proj(brt[1], brt[0], cc, ornT, kchunks=8)
                t_ = rot(scr)
                P.i("dve", "tensor_tensor", [ps, s_], [t_], out=t_[:], in0=ps[:, :GN], in1=s_[:], op=ALU.mult)
                P.i("pool", "tensor_tensor", [(h2f, c), t_], [(zT, c)], out=zT[:, c, :], in0=h2f[:, c, :], in1=t_[:], op=ALU.add)
        for cb in range(4):
            wb, wv = WP.load(wout_d, cb * 512, 512, KC)
            for cc in range(4):
                c = cb * 4 + cc
                ps = proj(wv, wb, cc, zT)
                for (o, n, isc) in rngs:
                    P.i("dve", "scalar_tensor_tensor", [ps, vec, (xg, c)], [(xg, c)], out=xg[:, c, o:o + n], in0=ps[:, o:o + n],
                        scalar=V(6 if isc else 5, c), in1=xg[:, c, o:o + n], op0=ALU.mult, op1=ALU.add)
        if P34_STAGE[0] == 3:
            raise _Stop()
        norm(rngs, 32, 9, 11, h2f, hb)
        for t in range(3):
            ps = R.get()
            for kc in range(KC):
                P.mm(ps[:, 0:36], h2f[:, kc, t * 128:(t + 1) * 128], wr[:, kc, :], kc == 0, kc == KC - 1, [wr] + hreads(h2f), [ps])
            lg = rt[:, 0, 0:36]
            key = (rt, t)
            P.i("dve", "tensor_tensor", [ps, rb], [key], out=lg, in0=ps[:, 0:36], in1=rb[:], op=ALU.add)
            m4 = rt[:, 1, 0:1]
            P.i("dve", "reduce_max", [key], [key], out=m4, in_=rt[:, 0, 0:4], axis=AX.X)
            oh4 = rt[:, 1, 4:8]
            P.i("dve", "tensor_scalar", [key], [key], out=oh4, in0=rt[:, 0, 0:4], scalar1=m4, scalar2=None, op0=ALU.is_ge)
            nm4 = rt[:, 1, 1:2]
            P.i("dve", "tensor_scalar", [key], [key], out=nm4, in0=m4, scalar1=-1.0, scalar2=None, op0=ALU.mult)
            e4 = rt[:, 1, 8:12]
            P.i("act", "activation", [key], [key], out=e4, in_=rt[:, 0, 0:4], func=AF.Exp, bias=nm4, scale=1.0)
            pg = rt[:, 1, 2:3]
            P.i("dve", "reduce_sum", [key], [key], out=pg, in_=e4, axis=AX.X)
            P.i("dve", "reciprocal", [key], [key], out=pg, in_=pg)
            pen = rt[:, 1, 12:16]
            P.i("dve", "tensor_scalar", [key], [key], out=pen, in0=oh4, scalar1=-1.0, scalar2=1e30, op0=ALU.add, op1=ALU.mult)
            lem = rt[:, 2, 0:32]
            for g4 in range(4):
                P.i("dve", "tensor_scalar", [key], [key], out=rt[:, 2, g4 * 8:(g4 + 1) * 8], in0=rt[:, 0, 4 + g4 * 8:12 + g4 * 8],
                    scalar1=rt[:, 1, 12 + g4:13 + g4], scalar2=None, op0=ALU.add)
            m1 = rt[:, 1, 16:17]
            P.i("dve", "reduce_max", [key], [key], out=m1, in_=lem, axis=AX.X)
            oh1 = rt[:, 3, 0:32]
            P.i("dve", "tensor_scalar", [key], [key], out=oh1, in0=lem, scalar1=m1, scalar2=None, op0=ALU.is_ge)
            lem2 = rt[:, 4, 0:32]
            P.i("dve", "scalar_tensor_tensor", [key], [key], out=lem2, in0=oh1, scalar=-1e30, in1=lem, op0=ALU.mult, op1=ALU.add)
            m2 = rt[:, 1, 17:18]
            P.i("dve", "reduce_max", [key], [key], out=m2, in_=lem2, axis=AX.X)
            oh2 = rt[:, 5, 0:32]
            P.i("dve", "tensor_scalar", [key], [key], out=oh2, in0=lem2, scalar1=m2, scalar2=None, op0=ALU.is_ge)
            r_ = rt[:, 1, 18:19]
            P.i("dve", "tensor_tensor", [key], [key], out=r_, in0=m2, in1=m1, op=ALU.subtract)
            P.i("act", "activation", [key], [key], out=r_, in_=r_, func=AF.Exp)
            den = rt[:, 1, 19:20]
            P.i("dve", "tensor_scalar", [key], [key], out=den, in0=r_, scalar1=1.0, scalar2=None, op0=ALU.add)
            P.i("dve", "reciprocal", [key], [key], out=den, in_=den)
            w1 = rt[:, 1, 20:21]
            P.i("dve", "tensor_tensor", [key], [key], out=w1, in0=den, in1=pg, op=ALU.mult)
            w2 = rt[:, 1, 21:22]
            P.i("dve", "tensor_tensor", [key], [key], out=w2, in0=w1, in1=r_, op=ALU.mult)
            G_ = rt[:, 6, 0:32]
            P.i("dve", "tensor_scalar", [key], [key], out=G_, in0=oh1, scalar1=w1, scalar2=None, op0=ALU.mult)
            P.i("dve", "scalar_tensor_tensor", [key], [key], out=G_, in0=oh2, scalar=w2, in1=G_, op0=ALU.mult, op1=ALU.add)
            ps2 = R.get()
            P.op("pe", lambda e, ps2=ps2, G_=G_: e.transpose(out=ps2[0:32, 0:128], in_=G_, identity=ident[:]), reads=[key, ident], writes=[ps2])
            P.i("act", "copy", [ps2], [(GT, t)], out=GT[:, t * 128:(t + 1) * 128], in_=ps2[0:32, 0:128])
        GTr = [(GT, t) for t in range(3)]
        ne = 32 if P34_STAGE[0] >= 5 else 2
        for e_ in range(ne):
            gm_ = rot(GTm)
            P.i("dve", "tensor_scalar", GTr + [ident], [gm_], out=gm_[:], in0=GT[:], scalar1=ident[0:32, e_:e_ + 1], scalar2=None, op0=ALU.mult)
            psb = R.get()
            P.mm(psb[:, :GN], ones[0:32, :], gm_[:], True, True, [gm_, ones], [psb])
            gb = rot(Gb)
            P.i("act", "copy", [psb], [gb], out=gb[:], in_=psb[:, :GN])
            wgb, wgv = WP.load(mg_d[e_], 0, 512, KC)
            wub, wuv_ = WP.load(mu_d[e_], 0, 512, KC)
            for hc in range(4):
                psg = proj(wgv, wgb, hc, hb)
                psu = proj(wuv_, wub, hc, hb)
                s_ = rot(scr)
                P.i("act", "activation", [psg], [s_], out=s_[:], in_=psg[:, :GN], func=AF.Silu)
                P.i("dve", "tensor_tensor", [psu, s_], [s_], out=s_[:], in0=psu[:, :GN], in1=s_[:], op=ALU.mult)
                P.i("pool", "tensor_tensor", [s_, gb], [(actT, hc)], out=actT[:, hc, :], in0=s_[:], in1=gb[:], op=ALU.mult)
            wdb, wdv = WP.load(md_d[e_], 0, 2048, 4)
            for c in range(KC):
                ps = proj(wdv, wdb, c, actT, kchunks=4)
                for (o, n, isc) in rngs:
                    P.i("dve", "scalar_tensor_tensor", [ps, vec, (xg, c)], [(xg, c)], out=xg[:, c, o:o + n], in0=ps[:, o:o + n],
                        scalar=V(13 if isc else 12, c), in1=xg[:, c, o:o + n], op0=ALU.mult, op1=ALU.add)

    def store(gi):
        xo_d = cur["xo"]
        c0 = gi * GN
        if final:
            ps = R.get()
            for kc in range(KC):
                s_ = rot(scr)
                P.i("act", "activation", [(xg, kc)], [s_], out=s_[:], in_=xg[:, kc, :], func=AF.Square)
                P.mm(ps[:, :GN], ones[:], s_[:], kc == 0, kc == KC - 1, [s_, ones], [ps])
            P.i("act", "activation", [ps], [rstd], out=rstd[:], in_=ps[:, :GN], func=AF.Sqrt, bias=EPS, scale=1.0 / D)
            P.i("dve", "reciprocal", [rstd], [rstd], out=rstd[:], in_=rstd[:])
            for kc in range(KC):
                P.i("dve", "scalar_tensor_tensor", [(xg, kc), vec, rstd], [(xg, kc)], out=xg[:, kc, :], in0=xg[:, kc, :], scalar=V(14, kc), in1=rstd[:],
                    op0=ALU.mult, op1=ALU.mult)
        for kc in range(KC):
            P.dma("sp", xo_d[kc * 128:(kc + 1) * 128, c0:c0 + GN], xg[:, kc, :], reads=[(xg, kc)])

    for v in range(nv):
        setup_v(v)
        for gi in range(3):
            try:
                group(gi)
            except _Stop:
                pass
            store(gi)
    return P


def prep_p34(xT_cores, p2res, l, inp, mod, mod_c, ident, final_w):
    ins = []
    w_in = inp["w_in"][l]
    w_uv = np.ascontiguousarray(w_in[:, 0:1024])
    w_post = np.ascontiguousarray(w_in[:, 4624:12304])
    gnw = np.ascontiguousarray(np.broadcast_to(inp["gm_norm_w"][l][None, :], (128, 512))).astype(np.float32)
    wsT = np.ascontiguousarray(inp["gm_spatial_w"][l].transpose(2, 0, 1).reshape(128, 512))
    bsb = np.ascontiguousarray(np.broadcast_to(inp["gm_spatial_b"][l].reshape(1, 512), (128, 512))).astype(np.float32)
    nrm = np.concatenate([inp["gdn_norm_w"][l].reshape(128, 1), colmajor(inp["ret_norm_w"][l])], axis=1)
    wr = np.ascontiguousarray(np.concatenate([inp["router_group_w"][l], inp["router_expert_w"][l]], axis=1))
    rb = np.concatenate([inp["router_group_b"][l], inp["router_expert_b"][l]])[None, :]
    rb = np.ascontiguousarray(np.broadcast_to(rb, (128, 36))).astype(np.float32)
    for core in range(8):
        b, half = core // 2, core % 2
        m = mod[b]
        sl = lambda a, i: a[i * D:(i + 1) * D]
        vecs = [inp["norm1_w"][l], sl(m, 1), sl(m, 0), sl(mod_c, 1), sl(mod_c, 0), sl(m, 2), sl(mod_c, 2),
                inp["norm2_w"][l], sl(m, 4), sl(m, 3), sl(mod_c, 4), sl(mod_c, 3), sl(m, 5), sl(mod_c, 5), final_w]
        vec = np.ascontiguousarray(np.concatenate([colmajor(v) for v in vecs], axis=1))
        f, bk = p2res[2 * b], p2res[2 * b + 1]
        og = np.stack([halves_from_seq(f["ogT"], 1, False)[half], halves_from_seq(bk["ogT"], 1, True)[half]])
        orr = np.stack([halves_from_seq(f["orT"], 1, False)[half], halves_from_seq(bk["orT"], 1, True)[half]])
        ins.append(dict(xT=xT_cores[core], vec=vec, w_uv=w_uv, w_post=w_post, og=np.ascontiguousarray(og), orr=np.ascontiguousarray(orr),
                        gnw=gnw, wsT=wsT, bsb=bsb, nrm=np.ascontiguousarray(nrm), wbr_gm=inp["w_br_gm"][l], wbr_gdn=inp["w_br_gdn"][l],
                        wbr_ret=inp["w_br_ret"][l], w_out=inp["w_out"][l], wr=wr, rb=rb, mg=inp["moe_w_gate"][l], mu=inp["moe_w_up"][l],
                        md=inp["moe_w_down"][l], ident=ident))
    return ins


def build_pm():
    P = Prog()
    nc = P.nc
    cin_d = nc.dram_tensor("cin", [128, KC * 5], F32, kind="ExternalInput").ap()
    w_d = nc.dram_tensor("modw", [DEPTH, D, 1536], F32, kind="ExternalInput").ap()
    b_d = nc.dram_tensor("modb", [DEPTH, 5, 1536], F32, kind="ExternalInput").ap()
    o_d = nc.dram_tensor("modo", [DEPTH, 5, 1536], F32, kind="ExternalOutput").ap()
    R = PsumRing(P)
    cin = P.sb([128, KC, 5], F32, name="cin_sb")
    wb = [P.sb([128, KC, 512], F32, name=f"mw{i}") for i in range(2)]
    bb = P.sb([5, DEPTH, 1536], F32, name="bb")
    ob = [P.sb([5, 512], F32, name=f"ob{i}") for i in range(2)]
    P.dma("sp", cin[:].rearrange("p a b -> p (a b)"), cin_d, writes=[cin])
    for l in range(DEPTH):
        P.dma("sp", bb[:, l, :], b_d[l], writes=[bb])
    P.i("act", "activation", [cin], [cin], out=cin[:], in_=cin[:], func=AF.Silu)
    k = 0
    for l in range(DEPTH):
        for cb in range(3):
            w = wb[k % 2]
            o = ob[k % 2]
            k += 1
            src = w_d[l][:, cb * 512:(cb + 1) * 512].rearrange("(kc p) n -> p kc n", p=128)
            for k0 in range(0, KC, 4):
                P.dma("sp", w[:, k0:k0 + 4, :], src[:, k0:k0 + 4, :], writes=[(w, k0)])
            ps = R.get()
            for kc in range(KC):
                P.mm(ps[0:5, :], cin[:, kc, :], w[:, kc, :], kc == 0, kc == KC - 1, [cin, (w, (kc // 4) * 4)], [ps])
            P.i("dve", "tensor_tensor", [ps, bb], [o], out=o[:], in0=ps[0:5, :], in1=bb[:, l, cb * 512:(cb + 1) * 512], op=ALU.add)
            P.dma("sp", o_d[l][:, cb * 512:(cb + 1) * 512], o[:], reads=[o])
    return P


import time as _time
import sys as _sys


def _log(*a):
    print("[kernel]", *a, file=_sys.stderr, flush=True)


def run1(P, in_map):
    t = _time.time()
    nc = P.finish()
    res = run_bass_kernel_spmd(nc, [in_map], core_ids=[0])
    _log("run1 done in", round(_time.time() - t, 1))
    return res.results[0]


def kernel(**inputs):
    inp = {k: np.ascontiguousarray(np.asarray(v, dtype=np.float32)) for k, v in inputs.items()}
    x, ctx = inp["x"], inp["ctx"]
    c5 = np.concatenate([inp["c"], inp["c_ctx"][None, :]], axis=0)
    cin = np.ascontiguousarray(c5.reshape(5, KC, 128).transpose(2, 1, 0).reshape(128, KC * 5))
    pm_in = []
    for core in range(8):
        sl = slice(core * 1536, (core + 1) * 1536)
        modb = np.ascontiguousarray(np.broadcast_to(inp["mod_b"][:, None, sl], (DEPTH, 5, 1536)))
        pm_in.append(dict(cin=cin, modw=np.ascontiguousarray(inp["mod_w"][:, :, sl]), modb=modb))
    pm = run_prog(build_pm(), pm_in)
    modall = np.concatenate([pm[c]["modo"] for c in range(8)], axis=2)
    cos, sin, perm = rope_tables()
    cos2 = np.ascontiguousarray(np.stack([cos[:, 0:1024], cos[:, 1024:2048]]))
    sin2 = np.ascontiguousarray(np.stack([sin[:, 0:1024], sin[:, 1024:2048]]))
    cst = p2_consts()
    ident = np.eye(128, dtype=np.float32)
    xT_cores = [core_tokens_T(x[c // 2], ctx[c // 2], c % 2) for c in range(8)]
    for l in range(DEPTH):
        mod, mod_c = modall[l, 0:4], modall[l, 4]
        vecs = []
        for core in range(8):
            b = core // 2
            vecs.append(np.concatenate([colmajor(inp["norm1_w"][l]), colmajor(mod[b, 2048:4096]), colmajor(mod[b, 0:2048]),
                                        colmajor(mod_c[2048:4096]), colmajor(mod_c[0:2048])], axis=1))
        p1 = run1(build_p1(8), dict(xT=np.stack(xT_cores), vec=np.stack(vecs), w_rec=np.ascontiguousarray(inp["w_in"][l][:, REC0:REC1]),
                                    cosT=cos2, sinT=sin2, perm=perm))
        p1res = [{k: p1[k][c] for k in ("qkvT", "abT", "rqT", "rkT", "rv")} for c in range(8)]
        t0_ = _time.time()
        p2res = run_prog(build_p2(), prep_p2(p1res, l, inp, cst))
        _log("layer", l, "p2 done in", round(_time.time() - t0_, 1))
        ins = prep_p34(xT_cores, p2res, l, inp, mod, mod_c, ident, inp["final_norm_w"])
        shared = {k: ins[0][k] for k in ins[0] if k not in ("xT", "vec", "og", "orr")}
        for k in ("xT", "vec", "og", "orr"):
            shared[k] = np.stack([ins[c][k] for c in range(8)])
        p34 = run1(build_p34(final=(l == DEPTH - 1), nv=8), shared)
        xT_cores = [np.ascontiguousarray(p34["xo"][c]) for c in range(8)]
    out = np.zeros((NB, SEQ, D), np.float32)
    for core in range(8):
        b, half = core // 2, core % 2
        out[b, half * 1024:(half + 1) * 1024] = xT_cores[core][:, 128:].T
    return out
```
